# Optimizing a Trainium2 kernel written in Bass

```python
import math
import jax, jax.numpy as jnp
from jax import lax
import numpy as np

D_MODEL = 1024
BATCH = 8
SEQ = 4096
DEPTH = 2

GRID_W = 64
CTX_LEN = 256
N_MIXERS = 2
N_HEADS = 16
N_KV_HEADS = 4
HEAD_DIM = D_MODEL // N_HEADS
GQA_GROUP = N_HEADS // N_KV_HEADS
Q_DIM = N_HEADS * HEAD_DIM
KV_DIM = N_KV_HEADS * HEAD_DIM
ROPE_AXIS_DIM = HEAD_DIM // 2
ROPE_THETA = 10000.0
ATTN_SCALE = HEAD_DIM ** -0.5
Q_BLOCK = 128
FOURIER_GROUPS = 4
N_GROUPS = 4
EXPERTS_PER_GROUP = 8
N_EXPERTS = N_GROUPS * EXPERTS_PER_GROUP
TOP_K_IN_GROUP = 2
D_EXPERT = D_MODEL // 2
MOE_BLOCK = 128
N_ATTN_LAYERS = (DEPTH + N_MIXERS - 1) // N_MIXERS
N_FOURIER_LAYERS = DEPTH // N_MIXERS
EPS = 1e-6

kernel_name = 'hybrid_attn_fourier_hmoe_dit'


def _rms_norm(x, gain):
    xf = x.astype(jnp.float32)
    y = xf * lax.rsqrt(jnp.mean(xf * xf, axis=-1, keepdims=True) + EPS)
    return y.astype(x.dtype) * gain


def _ada_params(silu_cond, w_mod, b_mod):
    return jnp.split(silu_cond @ w_mod + b_mod, 6, axis=-1)


def _modulate(x, gain, shift, scale):
    return _rms_norm(x, gain) * (1 + scale) + shift


def _axial_rope_tables(rows, dtype):
    half = ROPE_AXIS_DIM // 2
    inv = ROPE_THETA ** (-jnp.arange(half, dtype=jnp.float32) / half)
    ang_r = jnp.arange(rows, dtype=jnp.float32)[:, None] * inv
    ang_c = jnp.arange(GRID_W, dtype=jnp.float32)[:, None] * inv
    ang_r = jnp.broadcast_to(ang_r[:, None, :], (rows, GRID_W, half)).reshape(-1, half)
    ang_c = jnp.broadcast_to(ang_c[None, :, :], (rows, GRID_W, half)).reshape(-1, half)
    return (jnp.cos(ang_r).astype(dtype), jnp.sin(ang_r).astype(dtype),
            jnp.cos(ang_c).astype(dtype), jnp.sin(ang_c).astype(dtype))


def _rotate_half(x, cos, sin):
    shp = (cos.shape[0],) + (1,) * (x.ndim - 3) + (cos.shape[-1],)
    cos, sin = cos.reshape(shp), sin.reshape(shp)
    x1, x2 = jnp.split(x, 2, axis=-1)
    return jnp.concatenate([x1 * cos - x2 * sin, x2 * cos + x1 * sin], axis=-1)


def _apply_axial_rope(x, tables):
    cos_r, sin_r, cos_c, sin_c = tables
    return jnp.concatenate([_rotate_half(x[..., :ROPE_AXIS_DIM], cos_r, sin_r),
                            _rotate_half(x[..., ROPE_AXIS_DIM:], cos_c, sin_c)], axis=-1)


def _project_qkv(h, w_qkv, q_gain, k_gain, with_q):
    b, l, _ = h.shape
    if with_q:
        qkv = h @ w_qkv
        q = _rms_norm(qkv[..., :Q_DIM].reshape(b, l, N_KV_HEADS, GQA_GROUP, HEAD_DIM), q_gain)
        kv = qkv[..., Q_DIM:]
    else:
        q = None
        kv = h @ w_qkv[:, Q_DIM:]
    k = _rms_norm(kv[..., :KV_DIM].reshape(b, l, N_KV_HEADS, HEAD_DIM), k_gain)
    v = kv[..., KV_DIM:].reshape(b, l, N_KV_HEADS, HEAD_DIM)
    return q, k, v


def _attend(q, k, v):
    s = jnp.einsum('bqkgd,bskd->bkgqs', q, k).astype(jnp.float32) * ATTN_SCALE
    p = jax.nn.softmax(s, axis=-1).astype(v.dtype)
    return jnp.einsum('bkgqs,bskd->bqkgd', p, v)


def _gqa_attention(h_lat, h_ctx, w_qkv, q_gain, k_gain, w_o, rope, need_ctx_out):
    b, n, _ = h_lat.shape
    q_l, k_l, v_l = _project_qkv(h_lat, w_qkv, q_gain, k_gain, True)
    q_c, k_c, v_c = _project_qkv(h_ctx, w_qkv, q_gain, k_gain, need_ctx_out)
    q_l = _apply_axial_rope(q_l, rope)
    k_l = _apply_axial_rope(k_l, rope)
    k_all = jnp.concatenate([k_l, k_c], axis=1)
    v_all = jnp.concatenate([v_l, v_c], axis=1)
    n_blk = n // Q_BLOCK
    q_blocks = jnp.moveaxis(q_l.reshape(b, n_blk, Q_BLOCK, N_KV_HEADS, GQA_GROUP, HEAD_DIM), 1, 0)
    o = lax.map(lambda qb: _attend(qb, k_all, v_all), q_blocks)
    y_lat = jnp.moveaxis(o, 0, 1).reshape(b, n, Q_DIM) @ w_o
    y_ctx = None
    if need_ctx_out:
        y_ctx = _attend(q_c, k_c, v_c).reshape(b, h_ctx.shape[1], Q_DIM) @ w_o
    return y_lat, y_ctx


def _fourier_mix(h, w_o):
    b, n, d = h.shape
    hg = h.astype(jnp.float32).reshape(b, n, FOURIER_GROUPS, d // FOURIER_GROUPS)
    f = jnp.fft.fft2(hg, axes=(1, 3), norm='ortho').real
    return f.reshape(b, n, d).astype(h.dtype) @ w_o


def _hier_moe(h, w_rg, b_rg, w_re, b_re, w1, w3, w2):
    b, n, d = h.shape
    xt = h.reshape(-1, d)
    t = xt.shape[0]
    lg = (xt @ w_rg + b_rg).astype(jnp.float32)
    g_sel = jnp.argmax(lg, axis=-1)
    p_grp = jnp.max(jax.nn.softmax(lg, axis=-1), axis=-1)
    le = (xt @ w_re + b_re).astype(jnp.float32).reshape(t, N_GROUPS, EXPERTS_PER_GROUP)
    le_sel = jnp.take_along_axis(le, g_sel[:, None, None], axis=1)[:, 0]
    top_l, top_i = lax.top_k(le_sel, TOP_K_IN_GROUP)
    gate = p_grp[:, None] * jax.nn.softmax(top_l, axis=-1)
    eid = (g_sel[:, None] * EXPERTS_PER_GROUP + top_i).astype(jnp.int32)
    a = t * TOP_K_IN_GROUP
    flat_e = eid.reshape(-1)
    flat_t = jnp.arange(a, dtype=jnp.int32) // TOP_K_IN_GROUP
    flat_w = gate.reshape(-1).astype(h.dtype)
    order = jnp.argsort(flat_e)
    se = flat_e[order]
    counts = jnp.bincount(flat_e, length=N_EXPERTS)
    pcounts = (counts + MOE_BLOCK - 1) // MOE_BLOCK * MOE_BLOCK
    pend = jnp.cumsum(pcounts)
    pstart = pend - pcounts
    start = jnp.cumsum(counts) - counts
    dest = pstart[se] + jnp.arange(a, dtype=jnp.int32) - start[se]
    n_blk = (a + N_EXPERTS * (MOE_BLOCK - 1) + MOE_BLOCK - 1) // MOE_BLOCK
    p_rows = n_blk * MOE_BLOCK
    row_t = jnp.full((p_rows,), t, jnp.int32).at[dest].set(flat_t[order])
    row_w = jnp.zeros((p_rows,), h.dtype).at[dest].set(flat_w[order])
    blk_e = jnp.minimum(jnp.searchsorted(pend, jnp.arange(n_blk) * MOE_BLOCK, side='right'),
                        N_EXPERTS - 1).astype(jnp.int32)
    x_pad = jnp.concatenate([xt, jnp.zeros((1, d), xt.dtype)], axis=0)
    xb = x_pad[row_t].reshape(n_blk, MOE_BLOCK, d)

    def expert_block(args):
        xblk, e = args
        return (jax.nn.silu(xblk @ w1[e]) * (xblk @ w3[e])) @ w2[e]

    yb = lax.map(expert_block, (xb, blk_e)).reshape(p_rows, d)
    y = jax.ops.segment_sum(yb * row_w[:, None], row_t, num_segments=t + 1)[:t]
    return y.reshape(b, n, d)


def _context_needed_after(i):
    return any(j % N_MIXERS == 0 for j in range(i + 1, DEPTH))


def setup_inputs(seed: int = 0) -> dict:
    key = jax.random.key(seed)
    ks = jax.random.split(key, 21)
    nrm = lambda k, shape, s: jax.random.normal(k, shape, jnp.float32) * s
    return {
        'x': nrm(ks[0], (BATCH, SEQ, D_MODEL), 1.0),
        'c': nrm(ks[1], (BATCH, D_MODEL), 1.0),
        'ctx': nrm(ks[2], (BATCH, CTX_LEN, D_MODEL), 1.0),
        'c_ctx': nrm(ks[3], (D_MODEL,), 1.0),
        'w_mod': nrm(ks[4], (DEPTH, D_MODEL, 6 * D_MODEL), 0.5 * D_MODEL ** -0.5),
        'b_mod': nrm(ks[5], (DEPTH, 6 * D_MODEL), 0.02),
        'norm_mix': 1.0 + nrm(ks[6], (DEPTH, D_MODEL), 0.02),
        'norm_ffn': 1.0 + nrm(ks[7], (DEPTH, D_MODEL), 0.02),
        'attn_w_qkv': nrm(ks[8], (N_ATTN_LAYERS, D_MODEL, Q_DIM + 2 * KV_DIM), D_MODEL ** -0.5),
        'attn_q_norm': 1.0 + nrm(ks[9], (N_ATTN_LAYERS, HEAD_DIM), 0.02),
        'attn_k_norm': 1.0 + nrm(ks[10], (N_ATTN_LAYERS, HEAD_DIM), 0.02),
        'attn_w_o': nrm(ks[11], (N_ATTN_LAYERS, Q_DIM, D_MODEL), Q_DIM ** -0.5),
        'fourier_w_o': nrm(ks[12], (N_FOURIER_LAYERS, D_MODEL, D_MODEL), D_MODEL ** -0.5),
        'moe_w_rg': nrm(ks[13], (DEPTH, D_MODEL, N_GROUPS), D_MODEL ** -0.5),
        'moe_b_rg': nrm(ks[14], (DEPTH, N_GROUPS), 0.01),
        'moe_w_re': nrm(ks[15], (DEPTH, D_MODEL, N_EXPERTS), D_MODEL ** -0.5),
        'moe_b_re': nrm(ks[16], (DEPTH, N_EXPERTS), 0.01),
        'moe_w1': nrm(ks[17], (DEPTH, N_EXPERTS, D_MODEL, D_EXPERT), D_MODEL ** -0.5),
        'moe_w3': nrm(ks[18], (DEPTH, N_EXPERTS, D_MODEL, D_EXPERT), D_MODEL ** -0.5),
        'moe_w2': nrm(ks[19], (DEPTH, N_EXPERTS, D_EXPERT, D_MODEL), D_EXPERT ** -0.5),
        'final_norm': 1.0 + nrm(ks[20], (D_MODEL,), 0.02),
    }


def reference(x, c, ctx, c_ctx, w_mod, b_mod, norm_mix, norm_ffn, attn_w_qkv, attn_q_norm, attn_k_norm,
              attn_w_o, fourier_w_o, moe_w_rg, moe_b_rg, moe_w_re, moe_b_re, moe_w1, moe_w3, moe_w2,
              final_norm):
    n_tok = x.shape[1]
    rows = n_tok // GRID_W
    rope = _axial_rope_tables(rows, x.dtype)
    silu_c = jax.nn.silu(c)[:, None, :]
    silu_cc = jax.nn.silu(c_ctx)[None, None, :]
    s_ctx = ctx
    for i in range(DEPTH):
        kind = i % N_MIXERS
        j = i // N_MIXERS
        ctx_next = _context_needed_after(i)
        moe_p = (moe_w_rg[i], moe_b_rg[i], moe_w_re[i], moe_b_re[i], moe_w1[i], moe_w3[i], moe_w2[i])
        sh1, sc1, g1, sh2, sc2, g2 = _ada_params(silu_c, w_mod[i], b_mod[i])
        h = _modulate(x, norm_mix[i], sh1, sc1)
        if kind == 0 or ctx_next:
            csh1, csc1, cg1, csh2, csc2, cg2 = _ada_params(silu_cc, w_mod[i], b_mod[i])
            h_c = _modulate(s_ctx, norm_mix[i], csh1, csc1)
        if kind == 0:
            y, y_c = _gqa_attention(h, h_c, attn_w_qkv[j], attn_q_norm[j], attn_k_norm[j], attn_w_o[j],
                                    rope, ctx_next)
        else:
            y = _fourier_mix(h, fourier_w_o[j])
            y_c = _fourier_mix(h_c, fourier_w_o[j]) if ctx_next else None
        x = x + g1 * y
        x = x + g2 * _hier_moe(_modulate(x, norm_ffn[i], sh2, sc2), *moe_p)
        if ctx_next:
            s_ctx = s_ctx + cg1 * y_c
            s_ctx = s_ctx + cg2 * _hier_moe(_modulate(s_ctx, norm_ffn[i], csh2, csc2), *moe_p)
    return _rms_norm(x, final_norm)
```

```python
import contextlib
import os
import numpy as np
import ml_dtypes
import concourse.bass as bass
import concourse.mybir as mybir
from concourse.bass_utils import run_bass_kernel_spmd

F32 = mybir.dt.float32
BF16 = mybir.dt.bfloat16
I32 = mybir.dt.int32
U32 = mybir.dt.uint32
ALU = mybir.AluOpType
AF = mybir.ActivationFunctionType
AX = mybir.AxisListType

D = 1024
T = 4096
NT = 32
CTX = 256
NKT = 34
HD = 64
NH = 16
NKV = 4
NE = 32
DE = 512
EPS = 1e-6
SCALE = HD ** -0.5
STRICT = os.environ.get("SCHED_STRICT", "1") == "1"
MOD_F32R = os.environ.get("MOD_F32R", "0") == "1"


class Buf:
    def __init__(self, name, t=None, is_dram=False):
        self.name = name
        self.t = t
        self.is_dram = is_dram
        self.w = None
        self.rs = []

    def __getitem__(self, idx):
        return self.t[idx]


class Op:
    __slots__ = ("eng", "fn", "reads", "writes", "dma", "semkey", "deps", "need_inc", "val", "idx")


class Sched:
    ENG = ("pe", "act", "dve", "pool", "sp")

    def __init__(self, nc, stack):
        self.nc = nc
        self.gstack = stack
        self.stack = stack
        self.ops = []
        self.pos = 0
        self.e = {"pe": nc.tensor, "act": nc.scalar, "dve": nc.vector, "pool": nc.gpsimd, "sp": nc.sync}
        self.sems = {}
        self.cnt = {}
        self.seen = {k: {} for k in self.ENG}
        self.prefix = ""
        self.defer = None
        self.sempool = {"d_": [], "w_": []}
        self.allsems = []
        self.phase_keys = []
        self.final = {}
        for k in ("pe", "act", "dve", "pool"):
            self.getsem(k)

    def getsem(self, key):
        if key not in self.sems:
            if key[:2] in self.sempool and self.sempool[key[:2]]:
                sem, c = self.sempool[key[:2]].pop()
                self.sems[key] = sem
                self.cnt[key] = c
            else:
                self.sems[key] = self.gstack.enter_context(self.nc.semaphore("s%d" % len(self.allsems)))
                self.cnt[key] = 0
                self.allsems.append(self.sems[key])
            self.phase_keys.append(key)
        return self.sems[key]

    def recycle(self):
        for key in list(self.sems.keys()):
            if key[:2] in self.sempool:
                sem = self.sems.pop(key)
                c = self.cnt.pop(key)
                self.final[id(sem)] = (sem, c)
                self.sempool[key[:2]].append((sem, c))
                for en in self.ENG:
                    self.seen[en].pop(key, None)

    @contextlib.contextmanager
    def phase(self, name):
        old = (self.stack, self.prefix)
        with contextlib.ExitStack() as st:
            self.stack = st
            self.prefix = name + "_"
            yield
            self.barrier()
            self.emit()
            if old[0] is self.gstack:
                self.recycle()
        self.stack, self.prefix = old

    def sb(self, name, shape, dt):
        name = self.prefix + name
        t = self.stack.enter_context(self.nc.sbuf_tensor(name, list(shape), dt))
        return Buf(name, t)

    def ps(self, name, shape, dt):
        name = self.prefix + name
        t = self.stack.enter_context(self.nc.psum_tensor(name, list(shape), dt))
        return Buf(name, t)

    def dram(self, name, shape, dt, kind=None):
        if kind is None:
            t = self.nc.dram_tensor(name, list(shape), dt)
        else:
            t = self.nc.dram_tensor(name, list(shape), dt, kind=kind)
        return Buf(name, t.ap(), is_dram=True)

    def op(self, eng, fn, reads=(), writes=(), dma=False, semkey=None):
        if self.defer is not None:
            self.defer.append((eng, fn, list(reads), list(writes), dma, semkey))
            return None
        o = Op()
        o.eng = eng
        o.fn = fn
        o.reads = [b for b in reads if b is not None]
        o.writes = [b for b in writes if b is not None]
        o.dma = dma
        o.semkey = semkey
        o.deps = []
        o.need_inc = False
        o.val = None
        o.idx = len(self.ops)
        if dma and semkey is None:
            sbs = [b for b in (o.writes + o.reads) if not b.is_dram]
            o.semkey = ("w_" if eng == "pool" else "d_") + (sbs[0] if sbs else o.writes[0]).name
        o.reads = [b for b in o.reads if not b.is_dram]
        o.writes = [b for b in o.writes if not b.is_dram]
        deps = {}
        for b in o.reads:
            if b.w is not None:
                deps[b.w.idx] = (b.w, True)
        for b in o.writes:
            if b.w is not None and b.w.idx not in deps:
                deps[b.w.idx] = (b.w, False)
            for r in b.rs:
                if r.idx not in deps:
                    deps[r.idx] = (r, False)
        for (d, raw) in deps.values():
            if d is o:
                continue
            if (not d.dma) and d.eng == eng and not dma:
                if eng == "pe" or (not raw and not STRICT):
                    continue
            o.deps.append(d)
            d.need_inc = True
        for b in o.reads:
            b.rs.append(o)
        for b in o.writes:
            b.w = o
            b.rs = []
        self.ops.append(o)
        return o

    def replay(self, lst, n):
        for _ in range(min(n, len(lst))):
            eng, fn, r, w, d, k = lst.pop(0)
            self.op(eng, fn, reads=r, writes=w, dma=d, semkey=k)

    def interleave(self, fa, fb):
        la, lb = [], []
        self.defer = la
        if fa is not None:
            fa()
        self.defer = lb
        if fb is not None:
            fb()
        self.defer = None
        na, nb = max(len(la), 1), max(len(lb), 1)
        while la or lb:
            if la and (not lb or len(la) * nb >= len(lb) * na):
                self.replay(la, 1)
            else:
                self.replay(lb, 1)

    def barrier(self):
        last = {}
        for p in self.ops[self.pos:]:
            if p.eng != "barrier" and not p.dma:
                last[p.eng] = p
        for p in last.values():
            p.need_inc = True
        o = Op()
        o.eng = "barrier"
        o.idx = len(self.ops)
        o.deps = []
        o.dma = False
        o.need_inc = False
        o.val = None
        self.ops.append(o)

    def emit(self):
        cnt, sems, seen = self.cnt, self.sems, self.seen
        for o in self.ops[self.pos:]:
            if o.eng == "barrier":
                for en in self.ENG:
                    eng = self.e[en]
                    for key, c in cnt.items():
                        if c > 0 and seen[en].get(key, 0) < c and key != en:
                            eng.wait_ge(sems[key], c)
                            seen[en][key] = c
                continue
            eng = self.e[o.eng]
            need = {}
            for d in o.deps:
                if d.val is None:
                    continue
                key = d.semkey if d.dma else d.eng
                v = cnt[key] if d.dma else d.val
                if v > need.get(key, 0):
                    need[key] = v
            for key, v in need.items():
                if seen[o.eng].get(key, 0) >= v:
                    continue
                eng.wait_ge(sems[key], v)
                seen[o.eng][key] = v
            ins = o.fn(eng)
            if o.dma:
                s = self.getsem(o.semkey)
                cnt[o.semkey] += 16
                o.val = cnt[o.semkey]
                ins.then_inc(s, 16)
            elif o.need_inc:
                cnt[o.eng] += 1
                o.val = cnt[o.eng]
                ins.then_inc(sems[o.eng], 1)
            o.fn = None
        self.pos = len(self.ops)

    def finish(self):
        eng = self.e["sp"]
        for key, c in self.cnt.items():
            if c > 0 and self.seen["sp"].get(key, 0) < c:
                eng.wait_ge(self.sems[key], c)
                self.seen["sp"][key] = c
        for sem, c in self.final.values():
            if c > 0:
                eng.wait_ge(sem, c)


def dma(S, q, out_b, out_ap, in_b, in_ap):
    S.op(q, lambda e: e.dma_start(out=out_ap, in_=in_ap), reads=[in_b], writes=[out_b], dma=True)


def make_ident(S, dt):
    idf = S.sb("identf", [128, 128], F32)
    S.op("pool", lambda e: e.memset(idf[:, :], 0.0), writes=[idf])
    S.op("pool", lambda e: e.affine_select(out=idf[:, :], in_=idf[:, :], pattern=[[-1, 128]],
                                           compare_op=ALU.not_equal, fill=1.0, base=0, channel_multiplier=1),
         reads=[idf], writes=[idf])
    if dt == F32:
        return idf
    idb = S.sb("identb", [128, 128], BF16)
    S.op("dve", lambda e: e.tensor_copy(out=idb[:, :], in_=idf[:, :]), reads=[idf], writes=[idb])
    return idb


def bc_load(S, name, row_ap, n, src_buf, q="sp"):
    b = S.sb(name, [128, n], F32)
    S.op(q, lambda e: e.dma_start(out=b[:, :], in_=row_ap.to_broadcast([128, n])), reads=[src_buf], writes=[b], dma=True)
    return b


class Norm:
    def __init__(self, S, tag):
        self.S = S
        self.junk = S.sb(tag + "junk", [128, D], BF16)
        self.ss = [S.sb(tag + f"ss{i}", [128, 4], F32) for i in range(2)]
        self.tmp = [S.sb(tag + "tmp0", [128, D], F32)]
        self.i = 0

    def rstd(self, x, pool_ok=True):
        S = self.S
        ss = self.ss[self.i % 2]
        self.i += 1
        junk = self.junk
        S.op("pool" if pool_ok else "dve", lambda e: e.memset(ss[:, 0:1], 0.0), writes=[ss])
        S.op("act", lambda e: e.activation(out=junk[:, :], in_=x[:, :], func=AF.Square, accum_out=ss[:, 0:1]),
             reads=[x, ss], writes=[junk, ss])
        S.op("act", lambda e: e.activation(out=ss[:, 1:2], in_=ss[:, 0:1], func=AF.Sqrt, bias=EPS, scale=1.0 / D),
             reads=[ss], writes=[ss])
        S.op("dve", lambda e: e.reciprocal(out=ss[:, 2:3], in_=ss[:, 1:2]), reads=[ss], writes=[ss])
        return ss

    def apply(self, x, A, B, out, aoff=0, boff=0, pool_ok=True):
        S = self.S
        ss = self.rstd(x, pool_ok)
        tmp = self.tmp[0]
        S.op("dve", lambda e: e.scalar_tensor_tensor(out=tmp[:, :], in0=x[:, :], scalar=ss[:, 2:3], in1=A[:, aoff:aoff + D],
                                                     op0=ALU.mult, op1=ALU.mult), reads=[x, ss, A], writes=[tmp])
        if B is None:
            return tmp
        else:
            S.op("dve", lambda e: e.tensor_tensor(out=out[:, :], in0=tmp[:, :], in1=B[:, boff:boff + D], op=ALU.add),
                 reads=[tmp, B], writes=[out])


def transpose_to(S, src, ident, pT, dst, n, copy_eng, dst_ap=None):
    for k in range(n):
        S.op("pe", lambda e, k=k: e.transpose(out=pT[:, k, :], in_=src[:, k * 128:(k + 1) * 128], identity=ident[:, :]),
             reads=[src, ident], writes=[pT])
    oap = dst_ap if dst_ap is not None else dst[:, 0:n, :]
    if copy_eng == "act":
        S.op("act", lambda e: e.copy(out=oap, in_=pT[:, 0:n, :]), reads=[pT], writes=[dst])
    else:
        S.op(copy_eng, lambda e: e.tensor_copy(out=oap, in_=pT[:, 0:n, :]), reads=[pT], writes=[dst])


def phase_mods(S, io, layer, mods_d, modc_d):
    with S.phase(f"mod{layer}"):
        jobs = [(io["c"], mods_d, layer, 12)]
        if modc_d is not None:
            jobs.append((io["c_ctx"], modc_d, 0, 4))
        wb = [S.sb(f"w{i}", [128, 8, 512], F32) for i in range(2)]
        pm = [S.ps(f"pm{i}", [128, 512], F32) for i in range(2)]
        it = 0
        for ji, (cvec, dst, drow, nch) in enumerate(jobs):
            csb = S.sb(f"c{ji}", [128, 8], F32)
            S.op("sp", lambda e, csb=csb, cvec=cvec: e.dma_start(out=csb[:, :], in_=cvec.t.rearrange("(k p) -> p k", p=128),
                                                                 allow_slow_non_contiguous=True),
                 reads=[cvec], writes=[csb], dma=True)
            sc = S.sb(f"sc{ji}", [128, 8], F32)
            S.op("act", lambda e, sc=sc, csb=csb: e.activation(out=sc[:, :], in_=csb[:, :], func=AF.Silu), reads=[csb], writes=[sc])
            rep = S.sb(f"rep{ji}", [128, 8, 128], mybir.dt.float32r if MOD_F32R else F32)
            S.op("dve", lambda e, rep=rep, sc=sc: e.tensor_copy(out=rep[:, :, :], in_=sc[:, :].unsqueeze(2).to_broadcast([128, 8, 128])),
                 reads=[sc], writes=[rep])
            n = nch * 512
            mbc = bc_load(S, f"mbc{ji}", io["b_mod"].t[layer:layer + 1, 0:n], n, io["b_mod"])
            for ch in range(nch):
                w = wb[it % 2]
                p = pm[it % 2]
                it += 1
                S.op("sp", lambda e, w=w, ch=ch: e.dma_start(
                    out=w[:, :, :], in_=io["w_mod"].t[layer, :, ch * 512:(ch + 1) * 512].rearrange("(k p) n -> p k n", p=128)),
                    reads=[io["w_mod"]], writes=[w], dma=True)
                for k in range(8):
                    if MOD_F32R:
                        S.op("pe", lambda e, k=k, w=w, p=p, rep=rep: e.matmul(p[:, :], lhsT=rep[:, k, :], rhs=w[:, k, :].bitcast(mybir.dt.float32r),
                                                                              start=(k == 0), stop=(k == 7)), reads=[rep, w], writes=[p])
                    else:
                        S.op("pe", lambda e, k=k, w=w, p=p, rep=rep: e.matmul(p[:, :], lhsT=rep[:, k, :], rhs=w[:, k, :], start=(k == 0), stop=(k == 7)),
                             reads=[rep, w], writes=[p])
                S.op("dve", lambda e, p=p, ch=ch, mbc=mbc: e.tensor_tensor(out=mbc[:, ch * 512:(ch + 1) * 512], in0=p[:, :],
                                                                          in1=mbc[:, ch * 512:(ch + 1) * 512], op=ALU.add),
                     reads=[p, mbc], writes=[mbc])
            S.op("sp", lambda e, mbc=mbc, dst=dst, drow=drow, n=n: e.dma_start(out=dst.t[drow:drow + 1, 0:n], in_=mbc[0:1, 0:n]),
                 reads=[mbc], writes=[dst], dma=True)


def load_mod_tiles(S, mods_d, row, norm_d, nrow, which):
    off = which * 3 * D
    m = bc_load(S, f"m{which}", mods_d.t[row:row + 1, off:off + 3 * D], 3 * D, mods_d)
    g = bc_load(S, f"nrm{which}", norm_d.t[nrow:nrow + 1, :], D, norm_d)
    S.op("dve", lambda e: e.scalar_tensor_tensor(out=m[:, D:2 * D], in0=m[:, D:2 * D], scalar=1.0, in1=g[:, :],
                                                 op0=ALU.add, op1=ALU.mult), reads=[m, g], writes=[m])
    return m


def phase_attn(S, io, x_in, x_out, mods_d, modc_d):
    with S.phase("att"):
        ident = make_ident(S, BF16)
        m1 = load_mod_tiles(S, mods_d, 0, io["norm_mix"], 0, 0)
        qg = bc_load(S, "qg", io["attn_q_norm"].t[0:1, :], HD, io["attn_q_norm"])
        kg = bc_load(S, "kg", io["attn_k_norm"].t[0:1, :], HD, io["attn_k_norm"])
        wqkv = S.sb("wqkv", [128, 8, 1536], BF16)
        for c3 in range(3):
            S.op("pool", lambda e, c3=c3: e.dma_start(out=wqkv[:, :, c3 * 512:(c3 + 1) * 512],
                                                      in_=io["attn_w_qkv"].t[:, c3 * 512:(c3 + 1) * 512].rearrange("(k p) n -> p k n", p=128)),
                 reads=[io["attn_w_qkv"]], writes=[wqkv], dma=True)
        KT = S.sb("KT", [128, NKV, NKT * 128], BF16)
        for g in range(NKV):
            S.op("pool", lambda e, g=g: e.memset(KT[64:128, g, :], 0.0), writes=[KT])
        VA = S.sb("VA", [128, NKT, NKV, 128], BF16)
        S.op("pool", lambda e: e.memset(VA[:, :, :, 64:128], 1.0), writes=[VA])
        nrm = Norm(S, "n")
        hbf = [S.sb("h0", [128, D], BF16)] * 2
        hT = [S.sb("hT0", [128, 8, 128], BF16)] * 2
        pT = S.ps("pT", [128, 8, 128], BF16)
        pproj = S.ps("pproj", [128, 512], F32)
        sq = S.sb("sq", [128, 512], F32)
        st = [S.sb(f"st{i}", [128, 3, 8], F32) for i in range(2)]
        qn = [S.sb(f"qn{i}", [128, 512], F32) for i in range(2)]
        r1 = S.sb("r1", [128, 512], F32)
        r2 = S.sb("r2", [128, 512], F32)
        qbf = S.sb("qbf", [128, 512], BF16)
        cs = [S.sb(f"cs{i}", [128, 2, HD], F32) for i in range(2)]
        cnt = [0]

        def load_x(t, src, n_rows_off):
            b = xb[cnt[0] % 3]
            S.op("sp", lambda e: e.dma_start(out=b[:, :], in_=src.t[n_rows_off:n_rows_off + 128, :]), reads=[src], writes=[b], dma=True)
            return b

        def make_hT(x, A, B, aoff, boff):
            i = cnt[0]
            cnt[0] += 1
            h = hbf[i % 2]
            nrm.apply(x, A, B, h, aoff, boff)
            transpose_to(S, h, ident, pT, hT[i % 2], 8, "act")
            return hT[i % 2]

        def headnorm_rope(psrc, pcol, nh, gain, rope, outbf, ocol):
            w = nh * HD
            s = st[cnt[0] % 2]
            q = qn[cnt[0] % 2]
            S.op("act", lambda e: e.activation(out=sq[:, 0:w], in_=psrc[:, pcol:pcol + w], func=AF.Square), reads=[psrc], writes=[sq])
            S.op("dve", lambda e: e.tensor_reduce(out=s[:, 0, 0:nh], in_=sq[:, 0:w].rearrange("p (h d) -> p h d", d=HD), axis=AX.X, op=ALU.add),
                 reads=[sq], writes=[s])
            S.op("act", lambda e: e.activation(out=s[:, 1, 0:nh], in_=s[:, 0, 0:nh], func=AF.Sqrt, bias=EPS, scale=1.0 / HD), reads=[s], writes=[s])
            S.op("dve", lambda e: e.reciprocal(out=s[:, 2, 0:nh], in_=s[:, 1, 0:nh]), reads=[s], writes=[s])
            S.op("dve", lambda e: e.tensor_tensor(out=q[:, 0:w].rearrange("p (h d) -> p h d", d=HD),
                                                  in0=psrc[:, pcol:pcol + w].rearrange("p (h d) -> p h d", d=HD),
                                                  in1=s[:, 2, 0:nh].unsqueeze(2).to_broadcast([128, nh, HD]), op=ALU.mult),
                 reads=[psrc, s], writes=[q])
            if rope is None:
                S.op("pool", lambda e: e.tensor_tensor(out=outbf[:, ocol:ocol + w].rearrange("p (h d) -> p h d", d=HD),
                                                       in0=q[:, 0:w].rearrange("p (h d) -> p h d", d=HD),
                                                       in1=gain[:, :].unsqueeze(1).to_broadcast([128, nh, HD]), op=ALU.mult),
                     reads=[q, gain], writes=[outbf])
                return
            S.op("dve", lambda e: e.tensor_tensor(out=q[:, 0:w].rearrange("p (h d) -> p h d", d=HD),
                                                   in0=q[:, 0:w].rearrange("p (h d) -> p h d", d=HD),
                                                   in1=gain[:, :].unsqueeze(1).to_broadcast([128, nh, HD]), op=ALU.mult),
                 reads=[q, gain], writes=[q])
            S.op("dve", lambda e: e.tensor_tensor(out=r1[:, 0:w].rearrange("p (h d) -> p h d", d=HD),
                                                  in0=q[:, 0:w].rearrange("p (h d) -> p h d", d=HD),
                                                  in1=rope[:, 0, :].unsqueeze(1).to_broadcast([128, nh, HD]), op=ALU.mult),
                 reads=[q, rope], writes=[r1])
            for sidx in range(2):
                S.op("pool" if sidx == 0 else "dve", lambda e, sidx=sidx: e.tensor_tensor(
                    out=r2[:, 0:w].rearrange("p (h a s d) -> p h a s d", a=2, s=2, d=16)[:, :, :, sidx, :],
                    in0=q[:, 0:w].rearrange("p (h a s d) -> p h a s d", a=2, s=2, d=16)[:, :, :, 1 - sidx, :],
                    in1=rope[:, 1, :].rearrange("p (a s d) -> p a s d", a=2, s=2, d=16)[:, :, sidx, :].unsqueeze(1).to_broadcast([128, nh, 2, 16]),
                    op=ALU.mult), reads=[q, rope], writes=[r2])
            S.op("dve", lambda e: e.tensor_tensor(out=outbf[:, ocol:ocol + w], in0=r1[:, 0:w], in1=r2[:, 0:w], op=ALU.add),
                 reads=[r1, r2], writes=[outbf])

        def load_rope(t):
            c = cs[t % 2]
            S.op("sp", lambda e: e.dma_start(out=c[:, :, :], in_=io["rope"].t[:, t * 128:(t + 1) * 128, :].rearrange("c p d -> p c d")),
                 reads=[io["rope"]], writes=[c], dma=True)
            return c

        p1 = S.phase("att1")
        p1.__enter__()
        xb = [S.sb(f"x{i}", [128, D], F32) for i in range(3)]
        mc = bc_load(S, "mc", modc_d.t[0:1, 0:2 * D], 2 * D, modc_d)
        gmix = bc_load(S, "gmix", io["norm_mix"].t[0:1, :], D, io["norm_mix"])
        S.op("dve", lambda e: e.scalar_tensor_tensor(out=mc[:, D:2 * D], in0=mc[:, D:2 * D], scalar=1.0, in1=gmix[:, :],
                                                     op0=ALU.add, op1=ALU.mult), reads=[mc, gmix], writes=[mc])
        kbf = S.sb("kbf", [128, 256], BF16)
        pk = S.ps("pk", [64, NKV, 128], BF16)
        hbf1 = [hbf[0], S.sb("h1b", [128, D], BF16)]
        hT1 = [hT[0], S.sb("hT1b", [128, 8, 128], BF16)]
        fr = {}

        def front1(t):
            if t < NT:
                x = load_x(t, x_in, t * 128)
                A = m1
                rope = load_rope(t)
            else:
                x = load_x(t, io["ctx"], (t - NT) * 128)
                A = mc
                rope = None
            cnt[0] += 1
            h = hbf1[t % 2]
            nrm.apply(x, A, A, h, D, 0)
            transpose_to(S, h, ident, pT, hT1[t % 2], 8, "act")
            fr[t] = (hT1[t % 2], rope)

        def back1(t):
            hTt, rope = fr.pop(t)
            for k in range(8):
                S.op("pe", lambda e, k=k, hTt=hTt: e.matmul(pproj[:, :], lhsT=hTt[:, k, :], rhs=wqkv[:, k, 1024:1536], start=(k == 0), stop=(k == 7)),
                     reads=[hTt, wqkv], writes=[pproj])
            headnorm_rope(pproj, 0, NKV, kg, rope, kbf, 0)
            S.op("act", lambda e, t=t: e.copy(out=VA[:, t, :, 0:64], in_=pproj[:, 256:512].rearrange("p (g d) -> p g d", d=HD)),
                 reads=[pproj], writes=[VA])
            for g in range(NKV):
                S.op("pe", lambda e, g=g: e.transpose(out=pk[:, g, :], in_=kbf[:, g * 64:(g + 1) * 64], identity=ident[:, :]),
                     reads=[kbf, ident], writes=[pk])
            S.op("dve", lambda e, t=t: e.tensor_copy(out=KT[0:64, :, t * 128:(t + 1) * 128], in_=pk[:, :, :]), reads=[pk], writes=[KT])

        front1(0)
        for t in range(NKT):
            S.interleave((lambda t=t: front1(t + 1)) if t + 1 < NKT else None, lambda t=t: back1(t))
        p1.__exit__(None, None, None)
        p2 = S.phase("att2")
        p2.__enter__()
        wo = S.sb("wo", [128, NH, D], BF16)
        for hh in range(4):
            S.op("pool", lambda e, hh=hh: e.memset(wo[64:128, hh * 4:(hh + 1) * 4, :], 0.0), writes=[wo])
        for hh in range(2):
            S.op("pool", lambda e, hh=hh: e.dma_start(out=wo[0:64, hh * 8:(hh + 1) * 8, :],
                                                      in_=io["attn_w_o"].t[hh * 512:(hh + 1) * 512, :].rearrange("(h d) n -> d h n", d=64)),
                 reads=[io["attn_w_o"]], writes=[wo], dma=True)
        QC = 256
        NQC = T // QC
        QTs = [S.sb(f"QT{i}", [128, NH, QC], BF16) for i in range(2)]
        for i in range(2):
            S.op("pool", lambda e, i=i: e.memset(QTs[i][64:128, :, :], 0.0), writes=[QTs[i]])
        OT = S.sb("OT", [128, NH, QC], BF16)
        S.op("pool", lambda e: e.memset(OT[64:128, :, :], 0.0), writes=[OT])
        xq2 = [S.sb(f"xq{i}", [128, D], F32) for i in range(2)]
        xe = S.sb("xe", [128, D], F32)
        pS = [S.ps(f"pS{i}", [128, 4 * QC], F32) for i in range(2)]
        po = S.ps("po", [128, 4, QC], F32)
        pexp = [S.sb(f"pe{i}", [128, 4 * QC], BF16) for i in range(2)]
        rl = S.sb("rl", [64, 4, QC], F32)
        tmpy = sq
        NSTEP = NKV * NKT

        def prologue(qc):
            QT = QTs[qc % 2]
            for tt in range(2):
                t = qc * 2 + tt
                x = xq2[tt]
                S.op("sp", lambda e, x=x, t=t: e.dma_start(out=x[:, :], in_=x_in.t[t * 128:(t + 1) * 128, :]), reads=[x_in], writes=[x], dma=True)
                hTt = make_hT(x, m1, m1, D, 0)
                rope = load_rope(t)
                for half in range(2):
                    for k in range(8):
                        S.op("pe", lambda e, k=k, hTt=hTt, half=half: e.matmul(pproj[:, :], lhsT=hTt[:, k, :], rhs=wqkv[:, k, half * 512:(half + 1) * 512],
                                                                               start=(k == 0), stop=(k == 7)), reads=[hTt, wqkv], writes=[pproj])
                    headnorm_rope(pproj, 0, 8, qg, rope, qbf, 0)
                    for h8 in range(8):
                        S.op("pe", lambda e, h8=h8: e.transpose(out=pT[0:64, h8, :], in_=qbf[:, h8 * 64:(h8 + 1) * 64], identity=ident[:, :]),
                             reads=[qbf, ident], writes=[pT])
                    S.op("act", lambda e, half=half, tt=tt, QT=QT: e.copy(out=QT[0:64, half * 8:(half + 1) * 8, tt * 128:(tt + 1) * 128], in_=pT[0:64, :, :]),
                         reads=[pT], writes=[QT])

        def epilogue(qc):
            for tt in range(2):
                t = qc * 2 + tt
                xo_ = xe
                S.op("sp", lambda e, t=t: e.dma_start(out=xe[:, :], in_=x_in.t[t * 128:(t + 1) * 128, :]), reads=[x_in], writes=[xe], dma=True)
                for nch in range(2):
                    for h in range(NH):
                        S.op("pe", lambda e, h=h, nch=nch, tt=tt: e.matmul(pwo[:, :], lhsT=OTs[qc % 2][:, h, tt * 128:(tt + 1) * 128],
                                                                           rhs=wo[:, h, nch * 512:(nch + 1) * 512], start=(h == 0), stop=(h == NH - 1)),
                             reads=[OTs[qc % 2], wo], writes=[pwo])
                    S.op("dve", lambda e, nch=nch: e.tensor_tensor(out=tmpy[:, :], in0=pwo[:, :], in1=m1[:, 2 * D + nch * 512:2 * D + (nch + 1) * 512], op=ALU.mult),
                         reads=[pwo, m1], writes=[tmpy])
                    S.op("pool", lambda e, nch=nch, xo_=xo_: e.tensor_tensor(out=xo_[:, nch * 512:(nch + 1) * 512], in0=tmpy[:, :],
                                                                           in1=xo_[:, nch * 512:(nch + 1) * 512], op=ALU.add),
                         reads=[tmpy, xo_], writes=[xo_])
                S.op("pool", lambda e, t=t, xo_=xo_: e.dma_start(out=x_out.t[t * 128:(t + 1) * 128, :], in_=xo_[:, :]), reads=[xo_], writes=[x_out], dma=True)

        OTs = [OT, OT]
        pwo = pproj

        def emit_qk(qc, idx):
            g, kt = divmod(idx, NKT)
            ps_ = pS[idx % 2]
            QT = QTs[qc % 2]
            for j in range(2):
                S.op("pe", lambda e, j=j, g=g, kt=kt, ps_=ps_, QT=QT: e.matmul(ps_[:, j * 2 * QC:(j + 1) * 2 * QC], lhsT=KT[:, g, kt * 128:(kt + 1) * 128],
                                                                             rhs=QT[:, g * 4 + 2 * j:g * 4 + 2 * j + 2, :], start=True, stop=True),
                     reads=[KT, QT], writes=[ps_])

        prologue(0)
        for qc in range(NQC):
            pend = []
            S.defer = pend
            if qc >= 1:
                epilogue(qc - 1)
            if qc + 1 < NQC:
                prologue(qc + 1)
            S.defer = None
            OTc = OTs[qc % 2]
            emit_qk(qc, 0)
            for idx in range(NSTEP):
                g, kt = divmod(idx, NKT)
                ps_ = pS[idx % 2]
                px = pexp[idx % 2]
                if idx + 1 < NSTEP:
                    emit_qk(qc, idx + 1)
                S.op("act", lambda e, ps_=ps_, px=px: e.activation(out=px[:, :], in_=ps_[:, :], func=AF.Exp, scale=SCALE), reads=[ps_], writes=[px])
                for j in range(2):
                    S.op("pe", lambda e, j=j, g=g, kt=kt, px=px: e.matmul(po[:, 2 * j:2 * j + 2, :], lhsT=VA[:, kt, g, :], rhs=px[:, j * 2 * QC:(j + 1) * 2 * QC],
                                                                        start=(kt == 0), stop=(kt == NKT - 1)),
                         reads=[VA, px], writes=[po])
                if kt == NKT - 1:
                    S.op("dve", lambda e: e.reciprocal(out=rl[:, :, :], in_=po[64:128, :, :]), reads=[po], writes=[rl])
                    S.op("dve", lambda e, g=g, OTc=OTc: e.tensor_tensor(out=OTc[0:64, g * 4:(g + 1) * 4, :], in0=po[0:64, :, :], in1=rl[:, :, :], op=ALU.mult),
                         reads=[po, rl], writes=[OTc])
                S.replay(pend, 3 if idx < 28 else 2)
            S.replay(pend, len(pend))
        epilogue(NQC - 1)
        p2.__exit__(None, None, None)


def phase_moe(S, io, layer, x_in, x_out, mods_d, final_norm=False):
    HT = 16
    for half in range(2):
        with S.phase(f"moe{layer}{half}"):
            identf = make_ident(S, F32)
            identb = S.sb("idb", [128, 128], BF16)
            S.op("dve", lambda e: e.tensor_copy(out=identb[:, :], in_=identf[:, :]), reads=[identf], writes=[identb])
            m2 = load_mod_tiles(S, mods_d, layer, io["norm_ffn"], layer, 1)
            wr = S.sb("wr", [128, 8, 36], F32)
            S.op("sp", lambda e: e.dma_start(out=wr[:, :, 0:4], in_=io["moe_w_rg"].t[layer].rearrange("(k p) n -> p k n", p=128), allow_slow_non_contiguous=True),
                 reads=[io["moe_w_rg"]], writes=[wr], dma=True)
            S.op("sp", lambda e: e.dma_start(out=wr[:, :, 4:36], in_=io["moe_w_re"].t[layer].rearrange("(k p) n -> p k n", p=128), allow_slow_non_contiguous=True),
                 reads=[io["moe_w_re"]], writes=[wr], dma=True)
            br = S.sb("br", [128, 36], F32)
            S.op("sp", lambda e: e.dma_start(out=br[:, 0:4], in_=io["moe_b_rg"].t[layer:layer + 1, :].to_broadcast([128, 4])), reads=[io["moe_b_rg"]], writes=[br], dma=True)
            S.op("sp", lambda e: e.dma_start(out=br[:, 4:36], in_=io["moe_b_re"].t[layer:layer + 1, :].to_broadcast([128, 32])), reads=[io["moe_b_re"]], writes=[br], dma=True)
            nrm = Norm(S, "n")
            hTall = S.sb("hTall", [128, 8, HT * 128], BF16)
            G = S.sb("G", [128, HT, NE], F32)
            acc = S.sb("acc", [128, HT, D], F32)
            xb = [S.sb(f"x{i}", [128, D], F32) for i in range(2)]
            h2 = [S.sb(f"h2{i}", [128, D], F32) for i in range(2)]
            pTf = S.ps("pTf", [128, 8, 128], F32)
            hTf = S.sb("hTf", [128, 8, 128], F32)
            pl = S.ps("pl", [128, 512], F32)
            L = S.sb("L", [128, 36], F32)
            rt = S.sb("rt", [128, 16], F32)
            mg = S.sb("mg", [128, 4], F32)
            pen = S.sb("pen", [128, 4], F32)
            eg = S.sb("eg", [128, 4], F32)
            lem = S.sb("lem", [128, 32], F32)
            mx8 = S.sb("mx8", [128, 8], F32)
            sel = S.sb("sel", [128, 32], F32)
            ez = S.sb("ez", [128, 32], F32)
            MDBG = os.environ.get("MOE_DBG", "")
            if MDBG == "p2":
                S.op("pool", lambda e: e.memset(G[:, :, :], 1.0 / 32), writes=[G])
            for tt in range(HT):
                t = half * HT + tt
                x = xb[tt % 2]
                h = h2[tt % 2]
                if MDBG == "p1a0":
                    continue
                S.op("sp", lambda e, x=x, t=t: e.dma_start(out=x[:, :], in_=x_in.t[t * 128:(t + 1) * 128, :]), reads=[x_in], writes=[x], dma=True)
                nrm.apply(x, m2, m2, h, D, 0)
                if MDBG == "p1a1":
                    continue
                for k in range(8):
                    S.op("pe", lambda e, k=k, h=h: e.transpose(out=pTf[:, k, :], in_=h[:, k * 128:(k + 1) * 128], identity=identf[:, :]),
                         reads=[h, identf], writes=[pTf])
                S.op("act", lambda e: e.copy(out=hTf[:, :, :], in_=pTf[:, :, :]), reads=[pTf], writes=[hTf])
                if MDBG == "p1a2":
                    continue
                S.op("pool", lambda e, tt=tt: e.tensor_copy(out=hTall[:, :, tt * 128:(tt + 1) * 128], in_=hTf[:, :, :]), reads=[hTf], writes=[hTall])
                if MDBG in ("p2", "p1a", "p1a0", "p1a1", "p1a2"):
                    continue
                for k in range(8):
                    S.op("pe", lambda e, k=k: e.matmul(pl[:, 0:36], lhsT=hTf[:, k, :], rhs=wr[:, k, :], start=(k == 0), stop=(k == 7)),
                         reads=[hTf, wr], writes=[pl])
                S.op("dve", lambda e: e.tensor_tensor(out=L[:, :], in0=pl[:, 0:36], in1=br[:, :], op=ALU.add), reads=[pl, br], writes=[L])
                if MDBG == "p1b":
                    continue
                S.op("dve", lambda e: e.tensor_reduce(out=rt[:, 0:1], in_=L[:, 0:4], axis=AX.X, op=ALU.max), reads=[L], writes=[rt])
                S.op("dve", lambda e: e.tensor_scalar(out=mg[:, :], in0=L[:, 0:4], scalar1=rt[:, 0:1], scalar2=None, op0=ALU.is_ge), reads=[L, rt], writes=[mg])
                S.op("dve", lambda e: e.tensor_scalar(out=rt[:, 1:2], in0=rt[:, 0:1], scalar1=-1.0, scalar2=None, op0=ALU.mult), reads=[rt], writes=[rt])
                S.op("pool", lambda e: e.memset(rt[:, 2:3], 0.0), writes=[rt])
                S.op("act", lambda e: e.activation(out=eg[:, :], in_=L[:, 0:4], func=AF.Exp, bias=rt[:, 1:2], scale=1.0, accum_out=rt[:, 2:3]),
                     reads=[L, rt], writes=[eg, rt])
                S.op("dve", lambda e: e.reciprocal(out=rt[:, 3:4], in_=rt[:, 2:3]), reads=[rt], writes=[rt])
                S.op("dve", lambda e: e.tensor_scalar(out=pen[:, :], in0=mg[:, :], scalar1=-1.0, scalar2=1e30, op0=ALU.add, op1=ALU.mult), reads=[mg], writes=[pen])
                S.op("dve", lambda e: e.tensor_tensor(out=lem[:, :].rearrange("p (g j) -> p g j", j=8), in0=L[:, 4:36].rearrange("p (g j) -> p g j", j=8),
                                                      in1=pen[:, :].unsqueeze(2).to_broadcast([128, 4, 8]), op=ALU.add), reads=[L, pen], writes=[lem])
                S.op("dve", lambda e: e.max(out=mx8[:, :], in_=lem[:, :]), reads=[lem], writes=[mx8])
                S.op("dve", lambda e: e.tensor_scalar(out=sel[:, :], in0=lem[:, :], scalar1=mx8[:, 1:2], scalar2=None, op0=ALU.is_ge), reads=[lem, mx8], writes=[sel])
                S.op("dve", lambda e: e.tensor_scalar(out=rt[:, 4:5], in0=mx8[:, 0:1], scalar1=-1.0, scalar2=None, op0=ALU.mult), reads=[mx8], writes=[rt])
                S.op("act", lambda e: e.activation(out=ez[:, :], in_=lem[:, :], func=AF.Exp, bias=rt[:, 4:5], scale=1.0), reads=[lem, rt], writes=[ez])
                S.op("dve", lambda e: e.tensor_tensor(out=ez[:, :], in0=ez[:, :], in1=sel[:, :], op=ALU.mult), reads=[ez, sel], writes=[ez])
                S.op("dve", lambda e: e.tensor_reduce(out=rt[:, 5:6], in_=ez[:, :], axis=AX.X, op=ALU.add), reads=[ez], writes=[rt])
                S.op("dve", lambda e: e.reciprocal(out=rt[:, 6:7], in_=rt[:, 5:6]), reads=[rt], writes=[rt])
                S.op("dve", lambda e: e.tensor_tensor(out=rt[:, 7:8], in0=rt[:, 6:7], in1=rt[:, 3:4], op=ALU.mult), reads=[rt], writes=[rt])
                S.op("dve", lambda e, tt=tt: e.tensor_scalar(out=G[:, tt, :], in0=ez[:, :], scalar1=rt[:, 7:8], scalar2=None, op0=ALU.mult), reads=[ez, rt], writes=[G])
            w1b = [S.sb(f"w1_{i}", [128, 8, DE], BF16) for i in range(2)]
            w3b = [S.sb(f"w3_{i}", [128, 8, DE], BF16) for i in range(2)]
            w2b = [S.sb(f"w2_{i}", [128, 4, D], BF16) for i in range(2)]
            ph1 = S.ps("ph1", [128, DE], F32)
            ph3 = S.ps("ph3", [128, DE], F32)
            pa = S.ps("pa", [128, 8, 128], BF16)
            py = [S.ps(f"py{i}", [128, 512], F32) for i in range(2)]
            s1 = [S.sb(f"s1_{i}", [128, DE], F32) for i in range(2)]
            ab = [S.sb(f"ab{i}", [128, DE], BF16) for i in range(2)]
            aT = [S.sb(f"aT{i}", [128, 4, 128], BF16) for i in range(2)]
            for tt in range(HT):
                S.op("pool", lambda e, tt=tt: e.memset(acc[:, tt, :], 0.0), writes=[acc])
            it = 0
            for ex in range(NE if not MDBG.startswith("p1") else 0):
                w1, w3, w2 = w1b[ex % 2], w3b[ex % 2], w2b[ex % 2]
                S.op("pool", lambda e, w1=w1, ex=ex: e.dma_start(out=w1[:, :, :], in_=io["moe_w1"].t[layer, ex].rearrange("(k p) n -> p k n", p=128)),
                     reads=[io["moe_w1"]], writes=[w1], dma=True)
                S.op("pool", lambda e, w3=w3, ex=ex: e.dma_start(out=w3[:, :, :], in_=io["moe_w3"].t[layer, ex].rearrange("(k p) n -> p k n", p=128)),
                     reads=[io["moe_w3"]], writes=[w3], dma=True)
                for hh in range(2):
                    S.op("pool", lambda e, w2=w2, ex=ex, hh=hh: e.dma_start(out=w2[:, :, hh * 512:(hh + 1) * 512],
                                                                            in_=io["moe_w2"].t[layer, ex, :, hh * 512:(hh + 1) * 512].rearrange("(k p) n -> p k n", p=128)),
                         reads=[io["moe_w2"]], writes=[w2], dma=True)
                for tt in range(HT):
                    i2 = it % 2
                    it += 1
                    for k in range(8):
                        S.op("pe", lambda e, k=k, tt=tt, w1=w1: e.matmul(ph1[:, :], lhsT=hTall[:, k, tt * 128:(tt + 1) * 128], rhs=w1[:, k, :], start=(k == 0), stop=(k == 7)),
                             reads=[hTall, w1], writes=[ph1])
                    for k in range(8):
                        S.op("pe", lambda e, k=k, tt=tt, w3=w3: e.matmul(ph3[:, :], lhsT=hTall[:, k, tt * 128:(tt + 1) * 128], rhs=w3[:, k, :], start=(k == 0), stop=(k == 7)),
                             reads=[hTall, w3], writes=[ph3])
                    S.op("act", lambda e, i2=i2: e.activation(out=s1[i2][:, :], in_=ph1[:, :], func=AF.Silu), reads=[ph1], writes=[s1[i2]])
                    S.op("dve", lambda e, i2=i2: e.tensor_tensor(out=ab[i2][:, :], in0=ph3[:, :], in1=s1[i2][:, :], op=ALU.mult), reads=[ph3, s1[i2]], writes=[ab[i2]])
                    for fc in range(4):
                        S.op("pe", lambda e, fc=fc, i2=i2: e.transpose(out=pa[:, fc, :], in_=ab[i2][:, fc * 128:(fc + 1) * 128], identity=identb[:, :]),
                             reads=[ab[i2], identb], writes=[pa])
                    S.op("act", lambda e, i2=i2: e.copy(out=aT[i2][:, :, :], in_=pa[:, 0:4, :]), reads=[pa], writes=[aT[i2]])
                    for nch in range(2):
                        for fc in range(4):
                            S.op("pe", lambda e, fc=fc, nch=nch, i2=i2, w2=w2: e.matmul(py[nch][:, :], lhsT=aT[i2][:, fc, :], rhs=w2[:, fc, nch * 512:(nch + 1) * 512],
                                                                                       start=(fc == 0), stop=(fc == 3)), reads=[aT[i2], w2], writes=[py[nch]])
                        S.op("dve", lambda e, nch=nch, tt=tt, ex=ex: e.scalar_tensor_tensor(out=acc[:, tt, nch * 512:(nch + 1) * 512], in0=py[nch][:, :],
                                                                                            scalar=G[:, tt, ex:ex + 1], in1=acc[:, tt, nch * 512:(nch + 1) * 512],
                                                                                            op0=ALU.mult, op1=ALU.add), reads=[py[nch], G, acc], writes=[acc])
            if final_norm:
                fn = bc_load(S, "fn", io["final_norm"].t.rearrange("(o n) -> o n", o=1), D, io["final_norm"])
            for tt in range(HT):
                t = half * HT + tt
                x = xb[tt % 2]
                o = h2[tt % 2]
                S.op("sp", lambda e, x=x, t=t: e.dma_start(out=x[:, :], in_=x_in.t[t * 128:(t + 1) * 128, :]), reads=[x_in], writes=[x], dma=True)
                S.op("dve", lambda e, tt=tt, o=o: e.tensor_tensor(out=o[:, :], in0=acc[:, tt, :], in1=m2[:, 2 * D:3 * D], op=ALU.mult), reads=[acc, m2], writes=[o])
                S.op("pool", lambda e, o=o, x=x: e.tensor_tensor(out=o[:, :], in0=o[:, :], in1=x[:, :], op=ALU.add), reads=[o, x], writes=[o])
                if final_norm:
                    o = nrm.apply(o, fn, None, None, 0, 0)
                S.op("sp", lambda e, t=t, o=o: e.dma_start(out=x_out.t[t * 128:(t + 1) * 128, :], in_=o[:, :]), reads=[o], writes=[x_out], dma=True)


SLOT = 256
NBLK = 64


def phase_moe_sorted(S, io, layer, x_in, x_out, mods_d, scr, final_norm=False):
    h2_d, xs_d, yb_d = scr
    with S.phase(f"ms{layer}"):
        IDXi = S.sb("IDXi", [128, NT, 2], I32)
        GAB = S.sb("GAB", [128, NT, 2], F32)
        WIDXi = S.sb("WIDXi", [128, NBLK], I32)
        m2 = load_mod_tiles(S, mods_d, layer, io["norm_ffn"], layer, 1)
        pA = S.phase(f"msA{layer}")
        pA.__enter__()
        identf = make_ident(S, F32)
        wr = S.sb("wr", [128, 8, 36], F32)
        S.op("sp", lambda e: e.dma_start(out=wr[:, :, 0:4], in_=io["moe_w_rg"].t[layer].rearrange("(k p) n -> p k n", p=128), allow_slow_non_contiguous=True),
             reads=[io["moe_w_rg"]], writes=[wr], dma=True)
        S.op("sp", lambda e: e.dma_start(out=wr[:, :, 4:36], in_=io["moe_w_re"].t[layer].rearrange("(k p) n -> p k n", p=128), allow_slow_non_contiguous=True),
             reads=[io["moe_w_re"]], writes=[wr], dma=True)
        br = S.sb("br", [128, 36], F32)
        S.op("sp", lambda e: e.dma_start(out=br[:, 0:4], in_=io["moe_b_rg"].t[layer:layer + 1, :].to_broadcast([128, 4])), reads=[io["moe_b_rg"]], writes=[br], dma=True)
        S.op("sp", lambda e: e.dma_start(out=br[:, 4:36], in_=io["moe_b_re"].t[layer:layer + 1, :].to_broadcast([128, 32])), reads=[io["moe_b_re"]], writes=[br], dma=True)
        trif = S.sb("trif", [128, 128], F32)
        dma(S, "sp", trif, trif[:, :], io["tri"], io["tri"].t[:, :])
        trib = S.sb("trib", [128, 128], BF16)
        S.op("dve", lambda e: e.tensor_copy(out=trib[:, :], in_=trif[:, :]), reads=[trif], writes=[trib])
        onesb = S.sb("onesb", [128, 128], BF16)
        S.op("pool", lambda e: e.memset(onesb[:, :], 1.0), writes=[onesb])
        thr = bc_load(S, "thr", io["thr"].t[0:1, :], 16 + NBLK, io["thr"])
        pidx = S.sb("pidx", [128, 1], F32)
        dma(S, "sp", pidx, pidx[:, :], io["pidx"], io["pidx"].t[:, :])
        zt = S.sb("zt", [128, 8 * D], BF16)
        S.op("pool", lambda e: e.memset(zt[:, :], 0.0), writes=[zt])
        for z in range(NBLK * SLOT // 128 // 8):
            S.op("sp", lambda e, z=z: e.dma_start(out=xs_d.t.rearrange("(p r) n -> p (r n)", p=128)[:, z * 8 * D:(z + 1) * 8 * D], in_=zt[:, :]),
                 reads=[zt], writes=[xs_d], dma=True)
        nrm = Norm(S, "n")
        SEL = S.sb("SEL", [128, NT, NE], F32)
        G = S.sb("G", [128, NT, NE], F32)
        RANK = S.sb("RANK", [128, NT, NE], F32)
        tot = S.sb("tot", [128, NE], F32)
        S.op("pool", lambda e: e.memset(tot[:, :], 0.0), writes=[tot])
        xb = [S.sb(f"x{i}", [128, D], F32) for i in range(2)]
        h2 = [S.sb(f"h2{i}", [128, D], F32) for i in range(2)]
        h2b = [S.sb(f"h2b{i}", [128, D], BF16) for i in range(2)]
        pTf = S.ps("pTf", [128, 8, 128], F32)
        hTf = S.sb("hTf", [128, 8, 128], F32)
        pl = S.ps("pl", [128, 512], F32)
        prk = S.ps("prk", [128, 512], F32)
        L = S.sb("L", [128, 36], F32)
        rt = S.sb("rt", [128, 16], F32)
        mg = S.sb("mg", [128, 4], F32)
        pen = S.sb("pen", [128, 4], F32)
        eg = S.sb("eg", [128, 4], F32)
        lem = S.sb("lem", [128, 32], F32)
        mx8 = S.sb("mx8", [128, 8], F32)
        ez = S.sb("ez", [128, 32], F32)
        selb = S.sb("selb", [128, 32], BF16)
        pTf2 = S.ps("pTf2", [128, 8, 128], F32)
        pl2 = S.ps("pl2", [128, 512], F32)
        hTf2 = S.sb("hTf2", [128, 8, 128], F32)
        L2 = S.sb("L2", [128, 36], F32)
        pTfs, pls, hTfs, Ls = [pTf, pTf2], [pl, pl2], [hTf, hTf2], [L, L2]

        def front(tt):
            x = xb[tt % 2]
            h = h2[tt % 2]
            hb = h2b[tt % 2]
            pTf_, pl_, hTf_, L_ = pTfs[tt % 2], pls[tt % 2], hTfs[tt % 2], Ls[tt % 2]
            S.op("sp", lambda e, x=x, tt=tt: e.dma_start(out=x[:, :], in_=x_in.t[tt * 128:(tt + 1) * 128, :]), reads=[x_in], writes=[x], dma=True)
            nrm.apply(x, m2, m2, h, D, 0)
            S.op("pool", lambda e, h=h, hb=hb: e.tensor_copy(out=hb[:, :], in_=h[:, :]), reads=[h], writes=[hb])
            S.op("pool", lambda e, hb=hb, tt=tt: e.dma_start(out=h2_d.t[tt * 128:(tt + 1) * 128, :], in_=hb[:, :]), reads=[hb], writes=[h2_d], dma=True)
            for k in range(8):
                S.op("pe", lambda e, k=k, h=h: e.transpose(out=pTf_[:, k, :], in_=h[:, k * 128:(k + 1) * 128], identity=identf[:, :]),
                     reads=[h, identf], writes=[pTf_])
            S.op("act", lambda e: e.copy(out=hTf_[:, :, :], in_=pTf_[:, :, :]), reads=[pTf_], writes=[hTf_])
            for k in range(8):
                S.op("pe", lambda e, k=k: e.matmul(pl_[:, 0:36], lhsT=hTf_[:, k, :], rhs=wr[:, k, :], start=(k == 0), stop=(k == 7)),
                     reads=[hTf_, wr], writes=[pl_])
            S.op("dve", lambda e: e.tensor_tensor(out=L_[:, :], in0=pl_[:, 0:36], in1=br[:, :], op=ALU.add), reads=[pl_, br], writes=[L_])

        def back(tt, L):
            S.op("dve", lambda e: e.tensor_reduce(out=rt[:, 0:1], in_=L[:, 0:4], axis=AX.X, op=ALU.max), reads=[L], writes=[rt])
            S.op("dve", lambda e: e.tensor_scalar(out=mg[:, :], in0=L[:, 0:4], scalar1=rt[:, 0:1], scalar2=None, op0=ALU.is_ge), reads=[L, rt], writes=[mg])
            S.op("dve", lambda e: e.tensor_scalar(out=rt[:, 1:2], in0=rt[:, 0:1], scalar1=-1.0, scalar2=None, op0=ALU.mult), reads=[rt], writes=[rt])
            S.op("pool", lambda e: e.memset(rt[:, 2:3], 0.0), writes=[rt])
            S.op("act", lambda e: e.activation(out=eg[:, :], in_=L[:, 0:4], func=AF.Exp, bias=rt[:, 1:2], scale=1.0, accum_out=rt[:, 2:3]),
                 reads=[L, rt], writes=[eg, rt])
            S.op("dve", lambda e: e.reciprocal(out=rt[:, 3:4], in_=rt[:, 2:3]), reads=[rt], writes=[rt])
            S.op("dve", lambda e: e.tensor_scalar(out=pen[:, :], in0=mg[:, :], scalar1=-1.0, scalar2=1e30, op0=ALU.add, op1=ALU.mult), reads=[mg], writes=[pen])
            S.op("dve", lambda e: e.tensor_tensor(out=lem[:, :].rearrange("p (g j) -> p g j", j=8), in0=L[:, 4:36].rearrange("p (g j) -> p g j", j=8),
                                                  in1=pen[:, :].unsqueeze(2).to_broadcast([128, 4, 8]), op=ALU.add), reads=[L, pen], writes=[lem])
            S.op("dve", lambda e: e.max(out=mx8[:, :], in_=lem[:, :]), reads=[lem], writes=[mx8])
            S.op("dve", lambda e, tt=tt: e.tensor_scalar(out=SEL[:, tt, :], in0=lem[:, :], scalar1=mx8[:, 1:2], scalar2=None, op0=ALU.is_ge), reads=[lem, mx8], writes=[SEL])
            S.op("dve", lambda e: e.tensor_scalar(out=rt[:, 4:5], in0=mx8[:, 0:1], scalar1=-1.0, scalar2=None, op0=ALU.mult), reads=[mx8], writes=[rt])
            S.op("act", lambda e: e.activation(out=ez[:, :], in_=lem[:, :], func=AF.Exp, bias=rt[:, 4:5], scale=1.0), reads=[lem, rt], writes=[ez])
            S.op("dve", lambda e, tt=tt: e.tensor_tensor(out=ez[:, :], in0=ez[:, :], in1=SEL[:, tt, :], op=ALU.mult), reads=[ez, SEL], writes=[ez])
            S.op("dve", lambda e: e.tensor_reduce(out=rt[:, 5:6], in_=ez[:, :], axis=AX.X, op=ALU.add), reads=[ez], writes=[rt])
            S.op("dve", lambda e: e.reciprocal(out=rt[:, 6:7], in_=rt[:, 5:6]), reads=[rt], writes=[rt])
            S.op("dve", lambda e: e.tensor_tensor(out=rt[:, 7:8], in0=rt[:, 6:7], in1=rt[:, 3:4], op=ALU.mult), reads=[rt], writes=[rt])
            S.op("dve", lambda e, tt=tt: e.tensor_scalar(out=G[:, tt, :], in0=ez[:, :], scalar1=rt[:, 7:8], scalar2=None, op0=ALU.mult), reads=[ez, rt], writes=[G])
            S.op("pool", lambda e, tt=tt: e.tensor_copy(out=selb[:, :], in_=SEL[:, tt, :]), reads=[SEL], writes=[selb])
            S.op("pe", lambda e: e.matmul(prk[:, 0:32], lhsT=trib[:, :], rhs=selb[:, :], start=True, stop=True), reads=[trib, selb], writes=[prk])
            S.op("pe", lambda e: e.matmul(prk[:, 32:64], lhsT=onesb[:, :], rhs=selb[:, :], start=True, stop=True), reads=[onesb, selb], writes=[prk])
            S.op("dve", lambda e, tt=tt: e.tensor_tensor(out=RANK[:, tt, :], in0=prk[:, 0:32], in1=tot[:, :], op=ALU.add), reads=[prk, tot], writes=[RANK])
            S.op("dve", lambda e: e.tensor_tensor(out=tot[:, :], in0=prk[:, 32:64], in1=tot[:, :], op=ALU.add), reads=[prk, tot], writes=[tot])

        front(0)
        for tt in range(NT):
            S.interleave((lambda tt=tt: front(tt + 1)) if tt + 1 < NT else None, lambda tt=tt: back(tt, Ls[tt % 2]))
        cmp3 = S.sb("cmp3", [128, NE, 16], F32)
        nb = S.sb("nb", [128, NE], F32)
        sa = S.sb("sa", [128, NE], F32)
        sb_ = S.sb("sb_", [128, NE], F32)
        pc = S.sb("pc", [128, NE], F32)
        start = S.sb("start", [128, NE], F32)
        S.op("dve", lambda e: e.tensor_tensor(out=cmp3[:, :, :], in0=tot[:, :].unsqueeze(2).to_broadcast([128, NE, 16]),
                                              in1=thr[:, 0:16].unsqueeze(1).to_broadcast([128, NE, 16]), op=ALU.is_gt), reads=[tot, thr], writes=[cmp3])
        S.op("dve", lambda e: e.tensor_reduce(out=nb[:, :], in_=cmp3[:, :, :], axis=AX.X, op=ALU.add), reads=[cmp3], writes=[nb])
        S.op("dve", lambda e: e.tensor_scalar(out=pc[:, :], in0=nb[:, :], scalar1=float(SLOT), scalar2=None, op0=ALU.mult), reads=[nb], writes=[pc])
        S.op("dve", lambda e: e.tensor_copy(out=sa[:, :], in_=pc[:, :]), reads=[pc], writes=[sa])
        a, b = sa, sb_
        for sft in (1, 2, 4, 8, 16):
            S.op("dve", lambda e, a=a, b=b, sft=sft: e.tensor_tensor(out=b[:, sft:NE], in0=a[:, sft:NE], in1=a[:, 0:NE - sft], op=ALU.add), reads=[a], writes=[b])
            S.op("dve", lambda e, a=a, b=b, sft=sft: e.tensor_copy(out=b[:, 0:sft], in_=a[:, 0:sft]), reads=[a], writes=[b])
            a, b = b, a
        incl = a
        S.op("dve", lambda e: e.tensor_tensor(out=start[:, :], in0=incl[:, :], in1=pc[:, :], op=ALU.subtract), reads=[incl, pc], writes=[start])
        cmpj = S.sb("cmpj", [128, NBLK, NE], F32)
        ej = S.sb("ej", [128, NBLK], F32)
        S.op("dve", lambda e: e.tensor_tensor(out=cmpj[:, :, :], in0=incl[:, :].unsqueeze(1).to_broadcast([128, NBLK, NE]),
                                              in1=thr[:, 16:16 + NBLK].unsqueeze(2).to_broadcast([128, NBLK, NE]), op=ALU.is_le), reads=[incl, thr], writes=[cmpj])
        ejr = S.sb("ejr", [128, NBLK], F32)
        ejm = S.sb("ejm", [128, NBLK], F32)
        S.op("dve", lambda e: e.tensor_reduce(out=ejr[:, :], in_=cmpj[:, :, :], axis=AX.X, op=ALU.add), reads=[cmpj], writes=[ejr])
        S.op("dve", lambda e: e.tensor_copy(out=ej[:, :], in_=ejr[:, :]), reads=[ejr], writes=[ej])
        S.op("dve", lambda e: e.tensor_scalar(out=ej[:, :], in0=ej[:, :], scalar1=128.0, scalar2=pidx[:, 0:1], op0=ALU.mult, op1=ALU.add), reads=[ej, pidx], writes=[ej])
        S.op("dve", lambda e: e.tensor_scalar(out=ejm[:, :], in0=ejr[:, :], scalar1=float(NE) - 0.5, scalar2=1.0e6, op0=ALU.is_gt, op1=ALU.mult), reads=[ejr], writes=[ejm])
        ejs = S.sb("ejs", [128, NBLK], F32)
        S.op("dve", lambda e: e.memset(ejs[:, 0:1], 0.0), writes=[ejs])
        S.op("dve", lambda e: e.tensor_tensor(out=ejs[:, 1:NBLK], in0=ejr[:, 1:NBLK], in1=ejr[:, 0:NBLK - 1], op=ALU.is_equal), reads=[ejr], writes=[ejs])
        S.op("dve", lambda e: e.scalar_tensor_tensor(out=ejm[:, :], in0=ejs[:, :], scalar=1.0e6, in1=ejm[:, :], op0=ALU.mult, op1=ALU.add), reads=[ejs, ejm], writes=[ejm])
        S.op("dve", lambda e: e.scalar_tensor_tensor(out=ej[:, :], in0=ej[:, :], scalar=float(layer * NE * 128), in1=ejm[:, :], op0=ALU.add, op1=ALU.add),
             reads=[ej, ejm], writes=[ej])
        S.op("dve", lambda e: e.tensor_copy(out=WIDXi[:, :], in_=ej[:, :]), reads=[ej], writes=[WIDXi])
        md = S.sb("md", [128, NE], F32)
        eq = S.sb("eq", [128, NE], F32)
        idxf = S.sb("idxf", [128, NT, 2], F32)
        for tt in range(NT):
            S.op("dve", lambda e, tt=tt: e.tensor_tensor(out=md[:, :], in0=RANK[:, tt, :], in1=start[:, :], op=ALU.add), reads=[RANK, start], writes=[md])
            S.op("dve", lambda e, tt=tt: e.scalar_tensor_tensor(out=md[:, :], in0=md[:, :], scalar=1.0, in1=SEL[:, tt, :], op0=ALU.add, op1=ALU.mult),
                 reads=[md, SEL], writes=[md])
            S.op("dve", lambda e: e.max(out=mx8[:, :], in_=md[:, :]), reads=[md], writes=[mx8])
            S.op("dve", lambda e, tt=tt: e.tensor_scalar(out=idxf[:, tt, :], in0=mx8[:, 0:2], scalar1=-1.0, scalar2=None, op0=ALU.add), reads=[mx8], writes=[idxf])
            for ab in range(2):
                S.op("dve", lambda e, ab=ab: e.tensor_scalar(out=eq[:, :], in0=md[:, :], scalar1=mx8[:, ab:ab + 1], scalar2=None, op0=ALU.is_equal), reads=[md, mx8], writes=[eq])
                S.op("dve", lambda e, tt=tt: e.tensor_tensor(out=eq[:, :], in0=eq[:, :], in1=G[:, tt, :], op=ALU.mult), reads=[eq, G], writes=[eq])
                S.op("dve", lambda e, tt=tt, ab=ab: e.tensor_reduce(out=GAB[:, tt, ab:ab + 1], in_=eq[:, :], axis=AX.X, op=ALU.add), reads=[eq], writes=[GAB])
        S.op("dve", lambda e: e.tensor_copy(out=IDXi[:, :, :], in_=idxf[:, :, :]), reads=[idxf], writes=[IDXi])
        pA.__exit__(None, None, None)
        pB = S.phase(f"msB{layer}")
        pB.__enter__()
        hb2 = [S.sb(f"hb{i}", [128, D], BF16) for i in range(3)]
        for tt in range(NT):
            hb = hb2[tt % 3]
            S.op("sp", lambda e, hb=hb, tt=tt: e.dma_start(out=hb[:, :], in_=h2_d.t[tt * 128:(tt + 1) * 128, :]), reads=[h2_d], writes=[hb], dma=True)
            for ab in range(2):
                S.op("pool", lambda e, hb=hb, tt=tt, ab=ab: e.indirect_dma_start(out=xs_d.t[:, :], out_offset=bass.IndirectOffsetOnAxis(ap=IDXi[:, tt, ab:ab + 1], axis=0),
                                                                               in_=hb[:, :], in_offset=None),
                     reads=[hb, IDXi], writes=[xs_d], dma=True)
        pB.__exit__(None, None, None)
        pC = S.phase(f"msC{layer}")
        pC.__enter__()
        if getattr(S, "bc_reg", None) is None:
            S.bc_reg = S.gstack.enter_context(S.nc.gpsimd.register("bcreg"))
            S.op("pool", lambda e: e.reg_mov(S.bc_reg, 2 * NE * 128 - 1))
        identb = make_ident(S, BF16)
        w1s = [S.sb("w1s0", [128, 8 * DE], F32)] * 2
        w3s = [S.sb("w3s0", [128, 8 * DE], F32)] * 2
        w2s = [S.sb("w2s0", [128, 4 * D], F32)] * 2
        w1c = [S.sb(f"w1c{i}", [128, 8 * DE], BF16) for i in range(2)]
        w3c = [S.sb(f"w3c{i}", [128, 8 * DE], BF16) for i in range(2)]
        w2c = [S.sb(f"w2c{i}", [128, 4 * D], BF16) for i in range(2)]
        w1rows = io["moe_w1"].t.rearrange("l e (p r) n -> (l e p) (r n)", r=8)
        w3rows = io["moe_w3"].t.rearrange("l e (p r) n -> (l e p) (r n)", r=8)
        w2rows = io["moe_w2"].t.rearrange("l e (p r) n -> (l e p) (r n)", r=4)
        xs = [S.sb(f"xs{i}", [128, D], BF16) for i in range(2)]
        xT = [S.sb(f"xT{i}", [128, 8, 128], BF16) for i in range(2)]
        pT = S.ps("pT", [128, 8, 128], BF16)
        ph1 = S.ps("ph1", [128, DE], F32)
        ph3 = S.ps("ph3", [128, DE], F32)
        pa = S.ps("pa", [128, 8, 128], BF16)
        py = [S.ps(f"py{i}", [128, 512], F32) for i in range(2)]
        s1 = [S.sb(f"s1_{i}", [128, DE], F32) for i in range(2)]
        abf = [S.sb(f"ab{i}", [128, DE], BF16) for i in range(2)]
        aT = [S.sb(f"aT{i}", [128, 4, 128], BF16) for i in range(2)]
        yo = [S.sb(f"yo{i}", [128, D], F32) for i in range(2)]
        it = 0
        def gather_w(j):
            bi = j % 2
            for (rows, stg) in ((w1rows, w1s[bi]), (w3rows, w3s[bi]), (w2rows, w2s[bi])):
                S.op("pool", lambda e, rows=rows, stg=stg, j=j: e.indirect_dma_start(out=stg[:, :], out_offset=None, in_=rows,
                                                                                    in_offset=bass.IndirectOffsetOnAxis(ap=WIDXi[:, j:j + 1], axis=0),
                                                                                    bounds_check=S.bc_reg, oob_is_err=False),
                     reads=[WIDXi], writes=[stg], dma=True)

        def cast_w(j):
            bi = j % 2
            S.op("act", lambda e, bi=bi: e.copy(out=w1c[bi][:, :], in_=w1s[bi][:, :]), reads=[w1s[bi]], writes=[w1c[bi]])
            S.op("dve", lambda e, bi=bi: e.tensor_copy(out=w3c[bi][:, :], in_=w3s[bi][:, :]), reads=[w3s[bi]], writes=[w3c[bi]])
            S.op("dve", lambda e, bi=bi: e.tensor_copy(out=w2c[bi][:, :], in_=w2s[bi][:, :]), reads=[w2s[bi]], writes=[w2c[bi]])

        def load_xs(itx):
            x = xs[itx % 2]
            r0 = itx * 128
            S.op("sp", lambda e, x=x, r0=r0: e.dma_start(out=x[:, :], in_=xs_d.t[r0:r0 + 128, :]), reads=[xs_d], writes=[x], dma=True)

        gather_w(0)
        cast_w(0)
        load_xs(0)
        NSB = SLOT // 128
        NSUB = NBLK * NSB

        def stA(it):
            i2 = it % 2
            x = xs[i2]
            for r in range(8):
                S.op("pe", lambda e, r=r, x=x: e.transpose(out=pT[:, r, :], in_=x[:, :].rearrange("p (k r) -> p r k", r=8)[:, r, :], identity=identb[:, :]),
                     reads=[x, identb], writes=[pT])
            S.op("act", lambda e, i2=i2: e.copy(out=xT[i2][:, :, :], in_=pT[:, :, :]), reads=[pT], writes=[xT[i2]])

        def stB(it):
            i2 = it % 2
            bi = (it // NSB) % 2
            for r in range(8):
                S.op("pe", lambda e, r=r, i2=i2, bi=bi: e.matmul(ph1[:, :], lhsT=xT[i2][:, r, :], rhs=w1c[bi][:, r * DE:(r + 1) * DE], start=(r == 0), stop=(r == 7)),
                     reads=[xT[i2], w1c[bi]], writes=[ph1])
            for r in range(8):
                S.op("pe", lambda e, r=r, i2=i2, bi=bi: e.matmul(ph3[:, :], lhsT=xT[i2][:, r, :], rhs=w3c[bi][:, r * DE:(r + 1) * DE], start=(r == 0), stop=(r == 7)),
                     reads=[xT[i2], w3c[bi]], writes=[ph3])
            S.op("act", lambda e, i2=i2: e.activation(out=s1[i2][:, :], in_=ph1[:, :], func=AF.Silu), reads=[ph1], writes=[s1[i2]])
            S.op("dve", lambda e, i2=i2: e.tensor_tensor(out=abf[i2][:, :], in0=ph3[:, :], in1=s1[i2][:, :], op=ALU.mult), reads=[ph3, s1[i2]], writes=[abf[i2]])

        def stC(it):
            i2 = it % 2
            for r in range(4):
                S.op("pe", lambda e, r=r, i2=i2: e.transpose(out=pa[:, r, :], in_=abf[i2][:, :].rearrange("p (k r) -> p r k", r=4)[:, r, :], identity=identb[:, :]),
                     reads=[abf[i2], identb], writes=[pa])
            S.op("act", lambda e, i2=i2: e.copy(out=aT[i2][:, :, :], in_=pa[:, 0:4, :]), reads=[pa], writes=[aT[i2]])

        def stD(it):
            i2 = it % 2
            bi = (it // NSB) % 2
            y = yo[i2]
            r0 = it * 128
            for nch in range(2):
                for r in range(4):
                    S.op("pe", lambda e, r=r, nch=nch, i2=i2, bi=bi: e.matmul(py[nch][:, :], lhsT=aT[i2][:, r, :],
                                                                             rhs=w2c[bi][:, r * D + nch * 512:r * D + (nch + 1) * 512], start=(r == 0), stop=(r == 3)),
                         reads=[aT[i2], w2c[bi]], writes=[py[nch]])
                if nch == 0:
                    S.op("dve", lambda e, y=y, nch=nch: e.tensor_copy(out=y[:, nch * 512:(nch + 1) * 512], in_=py[nch][:, :]), reads=[py[nch]], writes=[y])
                else:
                    S.op("act", lambda e, y=y, nch=nch: e.copy(out=y[:, nch * 512:(nch + 1) * 512], in_=py[nch][:, :]), reads=[py[nch]], writes=[y])
            S.op("sp", lambda e, y=y, r0=r0: e.dma_start(out=yb_d.t[r0:r0 + 128, :], in_=y[:, :]), reads=[y], writes=[yb_d], dma=True)

        load_xs(1)
        stA(0)
        for it in range(NSUB):
            j, sblk = divmod(it, NSB)
            if it + 2 < NSUB:
                pass
            stB(it)
            if it >= 1:
                stD(it - 1)
            if it + 1 < NSUB:
                stA(it + 1)
                if it + 2 < NSUB:
                    load_xs(it + 2)
            if sblk == 0 and j + 1 < NBLK:
                gather_w(j + 1)
            stC(it)
            if sblk == NSB - 1 and j + 1 < NBLK:
                cast_w(j + 1)
        stD(NSUB - 1)
        pC.__exit__(None, None, None)
        pD = S.phase(f"msD{layer}")
        pD.__enter__()
        nrm2 = Norm(S, "n")
        if final_norm:
            fn = bc_load(S, "fn", io["final_norm"].t.rearrange("(o n) -> o n", o=1), D, io["final_norm"])
        xb2 = [S.sb(f"x{i}", [128, D], F32) for i in range(2)]
        ya = [S.sb(f"ya{i}", [128, D], F32) for i in range(3)]
        yb = [S.sb(f"yb{i}", [128, D], F32) for i in range(3)]
        for tt in range(NT):
            x = xb2[tt % 2]
            A = ya[tt % 3]
            B = yb[tt % 3]
            S.op("sp", lambda e, x=x, tt=tt: e.dma_start(out=x[:, :], in_=x_in.t[tt * 128:(tt + 1) * 128, :]), reads=[x_in], writes=[x], dma=True)
            for (ab, dst) in ((0, A), (1, B)):
                S.op("pool", lambda e, ab=ab, dst=dst, tt=tt: e.indirect_dma_start(out=dst[:, :], out_offset=None, in_=yb_d.t[:, :],
                                                                                  in_offset=bass.IndirectOffsetOnAxis(ap=IDXi[:, tt, ab:ab + 1], axis=0)),
                     reads=[IDXi], writes=[dst], dma=True)
            S.op("dve", lambda e, A=A, tt=tt: e.tensor_scalar(out=A[:, :], in0=A[:, :], scalar1=GAB[:, tt, 0:1], scalar2=None, op0=ALU.mult), reads=[A, GAB], writes=[A])
            S.op("dve", lambda e, A=A, B=B, tt=tt: e.scalar_tensor_tensor(out=A[:, :], in0=B[:, :], scalar=GAB[:, tt, 1:2], in1=A[:, :], op0=ALU.mult, op1=ALU.add),
                 reads=[A, B, GAB], writes=[A])
            S.op("dve", lambda e, A=A: e.tensor_tensor(out=A[:, :], in0=A[:, :], in1=m2[:, 2 * D:3 * D], op=ALU.mult), reads=[A, m2], writes=[A])
            S.op("dve", lambda e, A=A, x=x: e.tensor_tensor(out=A[:, :], in0=A[:, :], in1=x[:, :], op=ALU.add), reads=[A, x], writes=[A])
            o = A
            if final_norm:
                o = nrm2.apply(A, fn, None, None, 0, 0, pool_ok=False)
            S.op("act", lambda e, tt=tt, o=o: e.dma_start(out=x_out.t[tt * 128:(tt + 1) * 128, :], in_=o[:, :]), reads=[o], writes=[x_out], dma=True)
        pD.__exit__(None, None, None)


def phase_fourier(S, io, x_in, x_out, mods_d, y_d, f_d):
    with S.phase("fo1"):
        ident = make_ident(S, BF16)
        m1 = load_mod_tiles(S, mods_d, 1, io["norm_mix"], 1, 0)
        cc = S.sb("cc", [128, 2, 2, 256], BF16)
        for tb in range(2):
            S.op("pool", lambda e, tb=tb: e.dma_start(out=cc[:, tb, :, :], in_=io["dftc"].t[tb].rearrange("(k p) n -> p k n", p=128)),
                 reads=[io["dftc"]], writes=[cc], dma=True)
        nrm = Norm(S, "n")
        xb = [S.sb(f"x{i}", [128, D], F32) for i in range(2)]
        hbf = [S.sb(f"h{i}", [128, D], BF16) for i in range(2)]
        hT = [S.sb(f"hT{i}", [128, 8, 128], BF16) for i in range(2)]
        pT = S.ps("pT", [128, 8, 128], BF16)
        pyc = [S.ps(f"pyc{i}", [128, 512], F32) for i in range(4)]
        yb = [S.sb(f"yb{i}", [128, 2, D], BF16) for i in range(2)]
        def front_f1(t):
            x = xb[t % 2]
            S.op("sp", lambda e, x=x, t=t: e.dma_start(out=x[:, :], in_=x_in.t[t * 128:(t + 1) * 128, :]), reads=[x_in], writes=[x], dma=True)
            h = hbf[t % 2]
            nrm.apply(x, m1, m1, h, D, 0)
            transpose_to(S, h, ident, pT, hT[t % 2], 8, "act")
        def back_f1(t):
            y = yb[t % 2]
            for tb in range(2):
                for half in range(2):
                    p = pyc[tb * 2 + half]
                    for gg in range(2):
                        g = half * 2 + gg
                        for k2 in range(2):
                            S.op("pe", lambda e, p=p, gg=gg, g=g, k2=k2, tb=tb, t=t: e.matmul(p[:, gg * 256:(gg + 1) * 256], lhsT=hT[t % 2][:, g * 2 + k2, :],
                                                                                            rhs=cc[:, tb, k2, :], start=(k2 == 0), stop=(k2 == 1)),
                                 reads=[hT[t % 2], cc], writes=[p])
                    if half == 0:
                        S.op("act", lambda e, p=p, y=y, tb=tb, half=half: e.copy(out=y[:, tb, half * 512:(half + 1) * 512], in_=p[:, :]), reads=[p], writes=[y])
                    else:
                        S.op("dve", lambda e, p=p, y=y, tb=tb, half=half: e.tensor_copy(out=y[:, tb, half * 512:(half + 1) * 512], in_=p[:, :]), reads=[p], writes=[y])
            S.op("sp", lambda e, y=y, t=t: e.dma_start(out=y_d.t[:, t * 128:(t + 1) * 128, :].rearrange("c p n -> p c n"), in_=y[:, :, :]),
                 reads=[y], writes=[y_d], dma=True)
        front_f1(0)
        for t in range(NT):
            S.interleave((lambda t=t: front_f1(t + 1)) if t + 1 < NT else None, lambda t=t: back_f1(t))
    for cch in range(2):
        with S.phase(f"fo2{cch}"):
            Y = S.sb("Y", [128, 2, NT, 512], BF16)
            for tb in range(2):
                for q4 in range(4):
                    S.op("sp", lambda e, tb=tb, q4=q4: e.dma_start(
                        out=Y[:, tb, q4 * 8:(q4 + 1) * 8, :],
                        in_=y_d.t[tb, q4 * 1024:(q4 + 1) * 1024, cch * 512:(cch + 1) * 512].rearrange("(t p) n -> p t n", p=128)),
                        reads=[y_d], writes=[Y], dma=True)
            cn = [S.sb(f"cn{i}", [128, 2, NT, 128], BF16) for i in range(2)]
            pP = [S.ps(f"pP{i}", [128, 512], F32) for i in range(2)]
            pQ = [S.ps(f"pQ{i}", [128, 512], F32) for i in range(2)]
            Psb = [S.sb(f"Psb{i}", [128, 512], F32) for i in range(2)]
            fb = [S.sb(f"fb{i}", [128, 512], BF16) for i in range(2)]
            fm = [S.sb(f"fm{i}", [128, 512], BF16) for i in range(2)]
            NH2 = NT // 2
            jf = S.sb("jf", [128, 128], F32)
            S.op("pool", lambda e: e.memset(jf[:, :], 0.0), writes=[jf])
            S.op("pool", lambda e: e.affine_select(out=jf[:, :], in_=jf[:, :], pattern=[[1, 128]], compare_op=ALU.not_equal, fill=1.0, base=-127, channel_multiplier=1),
                 reads=[jf], writes=[jf])
            jb = S.sb("jb", [128, 128], BF16)
            S.op("dve", lambda e: e.tensor_copy(out=jb[:, :], in_=jf[:, :]), reads=[jf], writes=[jb])
            pJ = S.ps("pJ", [128, 512], F32)
            gr = [S.sb(f"gr{i}", [128, 512], BF16) for i in range(2)]

            def load_c(nt):
                c = cn[nt % 2]
                S.op("sp", lambda e, c=c, nt=nt: e.dma_start(out=c[:, :, 0:NT // 2 + 1, :], in_=io["dftn"].t[nt, :, :, 0:NT // 2 + 1, :]), reads=[io["dftn"]], writes=[c], dma=True)

            j1f = S.sb("j1f", [128, 128], F32)
            S.op("pool", lambda e: e.memset(j1f[:, :], 0.0), writes=[j1f])
            S.op("pool", lambda e: e.affine_select(out=j1f[:, :], in_=j1f[:, :], pattern=[[1, 128]], compare_op=ALU.not_equal, fill=1.0, base=-128, channel_multiplier=1),
                 reads=[j1f], writes=[j1f])
            j1 = S.sb("j1", [128, 128], BF16)
            S.op("dve", lambda e: e.tensor_copy(out=j1[:, :], in_=j1f[:, :]), reads=[j1f], writes=[j1])
            j2 = S.sb("j2", [128, 128], BF16)
            S.op("pool", lambda e: e.memset(j2[:, :], 0.0), writes=[j2])
            S.op("pool", lambda e: e.memset(j2[0:1, 0:1], 1.0), reads=[j2], writes=[j2])
            Yp = S.sb("Yp", [128, 2, NH2 + 1, 512], BF16)
            pM = S.ps("pM", [128, 512], F32)
            for t in range(NH2 + 1):
                for tb in range(2):
                    if t == NH2:
                        S.op("pe", lambda e, tb=tb, t=t: e.matmul(pM[:, :], lhsT=j2[:, :], rhs=Y[:, tb, t, :], start=True, stop=True), reads=[j2, Y], writes=[pM])
                        S.op("act", lambda e, tb=tb, t=t: e.copy(out=Yp[:, tb, t, :], in_=pM[:, :]), reads=[pM], writes=[Yp])
                        continue
                    S.op("pe", lambda e, tb=tb, t=t: e.matmul(pM[:, :], lhsT=j1[:, :], rhs=Y[:, tb, NT - 1 - t, :], start=True, stop=(t == 0)), reads=[j1, Y], writes=[pM])
                    if t >= 1:
                        S.op("pe", lambda e, tb=tb, t=t: e.matmul(pM[:, :], lhsT=j2[:, :], rhs=Y[:, tb, NT - t, :], start=False, stop=True), reads=[j2, Y], writes=[pM])
                    S.op("dve", lambda e, tb=tb, t=t: e.tensor_tensor(out=Yp[:, tb, t, :], in0=Y[:, tb, t, :], in1=pM[:, :],
                                                                      op=(ALU.add if tb == 0 else ALU.subtract)), reads=[Y, pM], writes=[Yp])
            NTC = NH2 + 1
            load_c(0)
            for nt in range(NH2 + 1):
                c = cn[nt % 2]
                if nt + 1 <= NH2:
                    load_c(nt + 1)
                p, q = pP[nt % 2], pQ[nt % 2]
                for t in range(NTC):
                    S.op("pe", lambda e, c=c, p=p, t=t: e.matmul(p[:, :], lhsT=c[:, 0, t, :], rhs=Yp[:, 0, t, :], start=(t == 0), stop=(t == NTC - 1)),
                         reads=[c, Yp], writes=[p])
                for t in range(NTC):
                    S.op("pe", lambda e, c=c, q=q, t=t: e.matmul(q[:, :], lhsT=c[:, 1, t, :], rhs=Yp[:, 1, t, :], start=(t == 0), stop=(t == NTC - 1)),
                         reads=[c, Yp], writes=[q])
                ps_ = Psb[nt % 2]
                f = fb[nt % 2]
                g = fm[nt % 2]
                S.op("act", lambda e, ps_=ps_, p=p: e.copy(out=ps_[:, :], in_=p[:, :]), reads=[p], writes=[ps_])
                S.op("dve", lambda e, ps_=ps_, q=q, f=f: e.tensor_tensor(out=f[:, :], in0=ps_[:, :], in1=q[:, :], op=ALU.add), reads=[ps_, q], writes=[f])
                if nt < NH2:
                    S.op("sp", lambda e, f=f, nt=nt: e.dma_start(out=f_d.t[nt * 128:(nt + 1) * 128, cch * 512:(cch + 1) * 512], in_=f[:, :]),
                         reads=[f], writes=[f_d], dma=True)
                    S.op("dve", lambda e, ps_=ps_, q=q, g=g: e.tensor_tensor(out=g[:, :], in0=ps_[:, :], in1=q[:, :], op=ALU.subtract), reads=[ps_, q], writes=[g])
                    g2 = gr[nt % 2]
                    S.op("pe", lambda e, g=g: e.matmul(pJ[:, :], lhsT=jb[:, :], rhs=g[:, :], start=True, stop=True), reads=[jb, g], writes=[pJ])
                    S.op("act", lambda e, g2=g2: e.copy(out=g2[:, :], in_=pJ[:, :]), reads=[pJ], writes=[g2])
                    npart = 127 if nt == 0 else 128
                    r0 = T - nt * 128 - 127
                    S.op("sp", lambda e, g2=g2, r0=r0, npart=npart: e.dma_start(out=f_d.t[r0:r0 + npart, cch * 512:(cch + 1) * 512], in_=g2[0:npart, :]),
                         reads=[g2], writes=[f_d], dma=True)
                else:
                    S.op("sp", lambda e, f=f, nt=nt: e.dma_start(out=f_d.t[nt * 128:nt * 128 + 1, cch * 512:(cch + 1) * 512], in_=f[0:1, :]),
                         reads=[f], writes=[f_d], dma=True)
    with S.phase("fo3"):
        ident = make_ident(S, BF16)
        m1 = load_mod_tiles(S, mods_d, 1, io["norm_mix"], 1, 0)
        wfo = S.sb("wfo", [128, 8, D], BF16)
        for hh in range(2):
            S.op("pool", lambda e, hh=hh: e.dma_start(out=wfo[:, :, hh * 512:(hh + 1) * 512],
                                                      in_=io["fourier_w_o"].t[:, hh * 512:(hh + 1) * 512].rearrange("(k p) n -> p k n", p=128)),
                 reads=[io["fourier_w_o"]], writes=[wfo], dma=True)
        fb = [S.sb(f"f{i}", [128, D], BF16) for i in range(2)]
        fT = [S.sb(f"fT{i}", [128, 8, 128], BF16) for i in range(2)]
        xb = [S.sb(f"x{i}", [128, D], F32) for i in range(2)]
        xo = [S.sb(f"xo{i}", [128, D], F32) for i in range(2)]
        pT = S.ps("pT", [128, 8, 128], BF16)
        py = [S.ps(f"py{i}", [128, 512], F32) for i in range(2)]
        tmpy = S.sb("tmpy", [128, 512], F32)
        def front_f3(t):
            f = fb[t % 2]
            x = xb[t % 2]
            S.op("sp", lambda e, f=f, t=t: e.dma_start(out=f[:, :], in_=f_d.t[t * 128:(t + 1) * 128, :]), reads=[f_d], writes=[f], dma=True)
            S.op("sp", lambda e, x=x, t=t: e.dma_start(out=x[:, :], in_=x_in.t[t * 128:(t + 1) * 128, :]), reads=[x_in], writes=[x], dma=True)
            transpose_to(S, f, ident, pT, fT[t % 2], 8, "act")
        def back_f3(t):
            x = xb[t % 2]
            o = xo[t % 2]
            for nch in range(2):
                for k in range(8):
                    S.op("pe", lambda e, k=k, nch=nch, t=t: e.matmul(py[nch][:, :], lhsT=fT[t % 2][:, k, :], rhs=wfo[:, k, nch * 512:(nch + 1) * 512],
                                                                     start=(k == 0), stop=(k == 7)), reads=[fT[t % 2], wfo], writes=[py[nch]])
                S.op("dve", lambda e, nch=nch: e.tensor_tensor(out=tmpy[:, :], in0=py[nch][:, :], in1=m1[:, 2 * D + nch * 512:2 * D + (nch + 1) * 512], op=ALU.mult),
                     reads=[py[nch], m1], writes=[tmpy])
                S.op("pool", lambda e, nch=nch, o=o, x=x: e.tensor_tensor(out=o[:, nch * 512:(nch + 1) * 512], in0=tmpy[:, :], in1=x[:, nch * 512:(nch + 1) * 512], op=ALU.add),
                     reads=[tmpy, x], writes=[o])
            S.op("sp", lambda e, t=t, o=o: e.dma_start(out=x_out.t[t * 128:(t + 1) * 128, :], in_=o[:, :]), reads=[o], writes=[x_out], dma=True)
        front_f3(0)
        for t in range(NT):
            S.interleave((lambda t=t: front_f3(t + 1)) if t + 1 < NT else None, lambda t=t: back_f3(t))


IN_SPECS = {
    "x": ([T, D], F32), "c": ([D], F32), "ctx": ([CTX, D], F32), "c_ctx": ([D], F32),
    "w_mod": ([2, D, 6 * D], F32), "b_mod": ([2, 6 * D], F32), "norm_mix": ([2, D], F32), "norm_ffn": ([2, D], F32),
    "attn_w_qkv": ([D, 1536], F32), "attn_q_norm": ([1, HD], F32), "attn_k_norm": ([1, HD], F32), "attn_w_o": ([D, D], F32),
    "fourier_w_o": ([D, D], F32), "moe_w_rg": ([2, D, 4], F32), "moe_b_rg": ([2, 4], F32), "moe_w_re": ([2, D, NE], F32),
    "moe_b_re": ([2, NE], F32), "moe_w1": ([2, NE, D, DE], F32), "moe_w3": ([2, NE, D, DE], F32), "moe_w2": ([2, NE, DE, D], F32),
    "final_norm": ([D], F32),
    "tri": ([128, 128], F32), "thr": ([1, 16 + 64], F32), "pidx": ([128, 1], F32),
    "rope": ([2, T, HD], F32), "dftc": ([2, 256, 256], F32), "dftn": ([NT, 128, 2, NT, 128], BF16),
}


def build_program(phases=("mod", "att", "moe0", "fou", "moe1"), only=None):
    nc = bass.Bass("TRN2", target_bir_lowering=False)
    with contextlib.ExitStack() as st:
        S = Sched(nc, st)
        io = {k: S.dram(k, shp, dt, kind="ExternalInput") for k, (shp, dt) in IN_SPECS.items() if only is None or k in only}
        out = S.dram("out", [T, D], F32, kind="ExternalOutput")
        mods_d = S.dram("mods_scr", [2, 6 * D], F32)
        modc_d = S.dram("modc_scr", [1, 2 * D], F32)
        x1 = S.dram("x1_scr", [T, D], F32)
        x2 = S.dram("x2_scr", [T, D], F32)
        x3 = S.dram("x3_scr", [T, D], F32)
        y_d = S.dram("y_scr", [2, T, D], BF16)
        f_d = S.dram("f_scr", [T, D], BF16)
        scr = (S.dram("h2_scr", [T, D], BF16), S.dram("xs_scr", [NBLK * SLOT, D], BF16), S.dram("yb_scr", [NBLK * SLOT, D], F32))
        DENSE = os.environ.get("MOE_DENSE", "0") == "1"
        last = phases[-1]
        if "mod" in phases:
            phase_mods(S, io, 0, mods_d, modc_d)
            phase_mods(S, io, 1, mods_d, None)
        if "att" in phases:
            phase_attn(S, io, io["x"], out if last == "att" else x1, mods_d, modc_d)
        if "moe0" in phases:
            if DENSE:
                phase_moe(S, io, 0, x1 if "att" in phases else io["x"], out if last == "moe0" else x2, mods_d)
            else:
                phase_moe_sorted(S, io, 0, x1 if "att" in phases else io["x"], out if last == "moe0" else x2, mods_d, scr)
        if "fou" in phases:
            phase_fourier(S, io, x2 if "moe0" in phases else io["x"], out if last == "fou" else x3, mods_d, y_d, f_d)
        if "moe1" in phases:
            if DENSE:
                phase_moe(S, io, 1, x3 if "fou" in phases else io["x"], out, mods_d, final_norm=True)
            else:
                phase_moe_sorted(S, io, 1, x3 if "fou" in phases else io["x"], out, mods_d, scr, final_norm=True)
        if last == "mod":
            with S.phase("dbg"):
                t = S.sb("t", [2, 6 * D], F32)
                dma(S, "sp", t, t[:, :], mods_d, mods_d.t[:, :])
                dma(S, "sp", out, out.t[0:12, :].rearrange("(a r) n -> a (r n)", a=2), t, t[:, :])
        S.finish()
    return nc


def host_constants():
    half = 16
    inv = (10000.0 ** (-np.arange(half, dtype=np.float32) / half)).astype(np.float32)
    n = np.arange(T)
    ang_r = (n // 64).astype(np.float32)[:, None] * inv
    ang_c = (n % 64).astype(np.float32)[:, None] * inv
    cr, sr, ccs, scs = np.cos(ang_r), np.sin(ang_r), np.cos(ang_c), np.sin(ang_c)
    cos64 = np.concatenate([cr, cr, ccs, ccs], axis=1)
    sin64 = np.concatenate([-sr, sr, -scs, scs], axis=1)
    rope = np.stack([cos64, sin64]).astype(np.float32)
    k = np.arange(256)
    a = 2 * np.pi * ((k[:, None] * k[None, :]) % 256) / 256.0
    dftc = np.stack([np.cos(a), np.sin(a)]).astype(np.float32) / 32.0
    m = np.arange(T, dtype=np.int64)
    an = 2 * np.pi * ((m[:, None] * m[None, :]) % T) / float(T)
    cn = (np.cos(an) / 32.0).astype(np.float32)
    sn = (-np.sin(an) / 32.0).astype(np.float32)
    tb = np.stack([cn, sn])
    tb = tb.reshape(2, NT, 128, NT, 128).transpose(3, 2, 0, 1, 4)
    dftn = np.ascontiguousarray(tb).astype(ml_dtypes.bfloat16)
    tri = (np.arange(128)[:, None] < np.arange(128)[None, :]).astype(np.float32)
    thr = np.concatenate([256.0 * np.arange(16), float(SLOT) * np.arange(NBLK)]).astype(np.float32)[None, :]
    pidx = np.arange(128, dtype=np.float32)[:, None]
    return {"rope": rope, "dftc": dftc, "dftn": dftn, "tri": tri, "thr": thr, "pidx": pidx}


def make_in_maps(inputs, consts):
    B = inputs["x"].shape[0]
    f = lambda a: np.ascontiguousarray(np.asarray(a, dtype=np.float32))
    shared = {
        "c_ctx": f(inputs["c_ctx"]), "w_mod": f(inputs["w_mod"]), "b_mod": f(inputs["b_mod"]),
        "norm_mix": f(inputs["norm_mix"]), "norm_ffn": f(inputs["norm_ffn"]), "attn_w_qkv": f(inputs["attn_w_qkv"][0]),
        "attn_q_norm": f(inputs["attn_q_norm"]), "attn_k_norm": f(inputs["attn_k_norm"]), "attn_w_o": f(inputs["attn_w_o"][0]),
        "fourier_w_o": f(inputs["fourier_w_o"][0]), "moe_w_rg": f(inputs["moe_w_rg"]), "moe_b_rg": f(inputs["moe_b_rg"]),
        "moe_w_re": f(inputs["moe_w_re"]), "moe_b_re": f(inputs["moe_b_re"]), "moe_w1": f(inputs["moe_w1"]),
        "moe_w3": f(inputs["moe_w3"]), "moe_w2": f(inputs["moe_w2"]), "final_norm": f(inputs["final_norm"]),
    }
    shared.update(consts)
    maps = []
    for b in range(B):
        m = dict(shared)
        m["x"] = f(inputs["x"][b])
        m["c"] = f(inputs["c"][b])
        m["ctx"] = f(inputs["ctx"][b])
        maps.append(m)
    return maps


def kernel(**inputs):
    consts = host_constants()
    maps = make_in_maps(inputs, consts)
    nc = build_program()
    res = run_bass_kernel_spmd(nc, maps, core_ids=list(range(len(maps))))
    return np.stack([np.asarray(r["out"], dtype=np.float32) for r in res.results], axis=0)
```

```python
import contextlib
import os
import numpy as np
import ml_dtypes
import concourse.bass as bass
import concourse.mybir as mybir
from concourse.bass_utils import run_bass_kernel_spmd

F32 = mybir.dt.float32
BF16 = mybir.dt.bfloat16
I32 = mybir.dt.int32
U32 = mybir.dt.uint32
ALU = mybir.AluOpType
AF = mybir.ActivationFunctionType
AX = mybir.AxisListType

D = 1024
T = 4096
NT = 32
CTX = 256
NKT = 34
HD = 64
NH = 16
NKV = 4
NE = 32
DE = 512
EPS = 1e-6
SCALE = HD ** -0.5
STRICT = os.environ.get("SCHED_STRICT", "0") == "1"
MOD_F32R = os.environ.get("MOD_F32R", "0") == "1"


class Buf:
    def __init__(self, name, t=None, is_dram=False):
        self.name = name
        self.t = t
        self.is_dram = is_dram
        self.w = None
        self.rs = []

    def __getitem__(self, idx):
        return self.t[idx]


class Op:
    __slots__ = ("eng", "fn", "reads", "writes", "dma", "semkey", "deps", "need_inc", "val", "idx")


class Sched:
    ENG = ("pe", "act", "dve", "pool", "sp")

    def __init__(self, nc, stack):
        self.nc = nc
        self.gstack = stack
        self.stack = stack
        self.ops = []
        self.pos = 0
        self.e = {"pe": nc.tensor, "act": nc.scalar, "dve": nc.vector, "pool": nc.gpsimd, "sp": nc.sync}
        self.sems = {}
        self.cnt = {}
        self.seen = {k: {} for k in self.ENG}
        self.prefix = ""
        self.defer = None
        self.sempool = {"d_": [], "w_": []}
        self.allsems = []
        self.phase_keys = []
        self.final = {}
        for k in ("pe", "act", "dve", "pool"):
            self.getsem(k)

    def getsem(self, key):
        if key not in self.sems:
            if key[:2] in self.sempool and self.sempool[key[:2]]:
                sem, c = self.sempool[key[:2]].pop()
                self.sems[key] = sem
                self.cnt[key] = c
            else:
                self.sems[key] = self.gstack.enter_context(self.nc.semaphore("s%d" % len(self.allsems)))
                self.cnt[key] = 0
                self.allsems.append(self.sems[key])
            self.phase_keys.append(key)
        return self.sems[key]

    def recycle(self):
        for key in list(self.sems.keys()):
            if key[:2] in self.sempool:
                sem = self.sems.pop(key)
                c = self.cnt.pop(key)
                self.final[id(sem)] = (sem, c)
                self.sempool[key[:2]].append((sem, c))
                for en in self.ENG:
                    self.seen[en].pop(key, None)

    @contextlib.contextmanager
    def phase(self, name):
        old = (self.stack, self.prefix)
        with contextlib.ExitStack() as st:
            self.stack = st
            self.prefix = name + "_"
            yield
            self.barrier()
            self.emit()
            if old[0] is self.gstack:
                self.recycle()
        self.stack, self.prefix = old

    def sb(self, name, shape, dt):
        name = self.prefix + name
        t = self.stack.enter_context(self.nc.sbuf_tensor(name, list(shape), dt))
        return Buf(name, t)

    def ps(self, name, shape, dt):
        name = self.prefix + name
        t = self.stack.enter_context(self.nc.psum_tensor(name, list(shape), dt))
        return Buf(name, t)

    def dram(self, name, shape, dt, kind=None):
        if kind is None:
            t = self.nc.dram_tensor(name, list(shape), dt)
        else:
            t = self.nc.dram_tensor(name, list(shape), dt, kind=kind)
        return Buf(name, t.ap(), is_dram=True)

    def op(self, eng, fn, reads=(), writes=(), dma=False, semkey=None):
        if self.defer is not None:
            self.defer.append((eng, fn, list(reads), list(writes), dma, semkey))
            return None
        o = Op()
        o.eng = eng
        o.fn = fn
        o.reads = [b for b in reads if b is not None]
        o.writes = [b for b in writes if b is not None]
        o.dma = dma
        o.semkey = semkey
        o.deps = []
        o.need_inc = False
        o.val = None
        o.idx = len(self.ops)
        if dma and semkey is None:
            sbs = [b for b in (o.writes + o.reads) if not b.is_dram]
            o.semkey = ("w_" if eng == "pool" else "d_") + (sbs[0] if sbs else o.writes[0]).name
        o.reads = [b for b in o.reads if not b.is_dram]
        o.writes = [b for b in o.writes if not b.is_dram]
        deps = {}
        for b in o.reads:
            if b.w is not None:
                deps[b.w.idx] = (b.w, True)
        for b in o.writes:
            if b.w is not None and b.w.idx not in deps:
                deps[b.w.idx] = (b.w, False)
            for r in b.rs:
                if r.idx not in deps:
                    deps[r.idx] = (r, False)
        for (d, raw) in deps.values():
            if d is o:
                continue
            if (not d.dma) and d.eng == eng and not dma:
                if eng == "pe" or (not raw and not STRICT):
                    continue
            o.deps.append(d)
            d.need_inc = True
        for b in o.reads:
            b.rs.append(o)
        for b in o.writes:
            b.w = o
            b.rs = []
        self.ops.append(o)
        return o

    def replay(self, lst, n):
        for _ in range(min(n, len(lst))):
            eng, fn, r, w, d, k = lst.pop(0)
            self.op(eng, fn, reads=r, writes=w, dma=d, semkey=k)

    def interleave(self, fa, fb):
        la, lb = [], []
        self.defer = la
        if fa is not None:
            fa()
        self.defer = lb
        if fb is not None:
            fb()
        self.defer = None
        na, nb = max(len(la), 1), max(len(lb), 1)
        while la or lb:
            if la and (not lb or len(la) * nb >= len(lb) * na):
                self.replay(la, 1)
            else:
                self.replay(lb, 1)

    def barrier(self):
        last = {}
        for p in self.ops[self.pos:]:
            if p.eng != "barrier" and not p.dma:
                last[p.eng] = p
        for p in last.values():
            p.need_inc = True
        o = Op()
        o.eng = "barrier"
        o.idx = len(self.ops)
        o.deps = []
        o.dma = False
        o.need_inc = False
        o.val = None
        self.ops.append(o)

    def emit(self):
        cnt, sems, seen = self.cnt, self.sems, self.seen
        for o in self.ops[self.pos:]:
            if o.eng == "barrier":
                for en in self.ENG:
                    eng = self.e[en]
                    for key, c in cnt.items():
                        if c > 0 and seen[en].get(key, 0) < c and key != en:
                            eng.wait_ge(sems[key], c)
                            seen[en][key] = c
                continue
            eng = self.e[o.eng]
            need = {}
            for d in o.deps:
                if d.val is None:
                    continue
                key = d.semkey if d.dma else d.eng
                v = cnt[key] if d.dma else d.val
                if v > need.get(key, 0):
                    need[key] = v
            for key, v in need.items():
                if seen[o.eng].get(key, 0) >= v:
                    continue
                eng.wait_ge(sems[key], v)
                seen[o.eng][key] = v
            ins = o.fn(eng)
            if o.dma:
                s = self.getsem(o.semkey)
                cnt[o.semkey] += 16
                o.val = cnt[o.semkey]
                ins.then_inc(s, 16)
            elif o.need_inc:
                cnt[o.eng] += 1
                o.val = cnt[o.eng]
                ins.then_inc(sems[o.eng], 1)
            o.fn = None
        self.pos = len(self.ops)

    def finish(self):
        eng = self.e["sp"]
        for key, c in self.cnt.items():
            if c > 0 and self.seen["sp"].get(key, 0) < c:
                eng.wait_ge(self.sems[key], c)
                self.seen["sp"][key] = c
        for sem, c in self.final.values():
            if c > 0:
                eng.wait_ge(sem, c)


def dma(S, q, out_b, out_ap, in_b, in_ap):
    S.op(q, lambda e: e.dma_start(out=out_ap, in_=in_ap), reads=[in_b], writes=[out_b], dma=True)


def make_ident(S, dt):
    idf = S.sb("identf", [128, 128], F32)
    S.op("pool", lambda e: e.memset(idf[:, :], 0.0), writes=[idf])
    S.op("pool", lambda e: e.affine_select(out=idf[:, :], in_=idf[:, :], pattern=[[-1, 128]],
                                           compare_op=ALU.not_equal, fill=1.0, base=0, channel_multiplier=1),
         reads=[idf], writes=[idf])
    if dt == F32:
        return idf
    idb = S.sb("identb", [128, 128], BF16)
    S.op("dve", lambda e: e.tensor_copy(out=idb[:, :], in_=idf[:, :]), reads=[idf], writes=[idb])
    return idb


def bc_load(S, name, row_ap, n, src_buf, q="sp"):
    b = S.sb(name, [128, n], F32)
    S.op(q, lambda e: e.dma_start(out=b[:, :], in_=row_ap.to_broadcast([128, n])), reads=[src_buf], writes=[b], dma=True)
    return b


class Norm:
    def __init__(self, S, tag):
        self.S = S
        self.junk = S.sb(tag + "junk", [128, D], BF16)
        self.ss = [S.sb(tag + f"ss{i}", [128, 4], F32) for i in range(2)]
        self.tmp = [S.sb(tag + "tmp0", [128, D], F32)]
        self.i = 0

    def rstd(self, x, pool_ok=True):
        S = self.S
        ss = self.ss[self.i % 2]
        self.i += 1
        junk = self.junk
        S.op("pool" if pool_ok else "dve", lambda e: e.memset(ss[:, 0:1], 0.0), writes=[ss])
        S.op("act", lambda e: e.activation(out=junk[:, :], in_=x[:, :], func=AF.Square, accum_out=ss[:, 0:1]),
             reads=[x, ss], writes=[junk, ss])
        S.op("act", lambda e: e.activation(out=ss[:, 1:2], in_=ss[:, 0:1], func=AF.Sqrt, bias=EPS, scale=1.0 / D),
             reads=[ss], writes=[ss])
        S.op("dve", lambda e: e.reciprocal(out=ss[:, 2:3], in_=ss[:, 1:2]), reads=[ss], writes=[ss])
        return ss

    def apply(self, x, A, B, out, aoff=0, boff=0, pool_ok=True):
        S = self.S
        ss = self.rstd(x, pool_ok)
        tmp = self.tmp[0]
        S.op("dve", lambda e: e.scalar_tensor_tensor(out=tmp[:, :], in0=x[:, :], scalar=ss[:, 2:3], in1=A[:, aoff:aoff + D],
                                                     op0=ALU.mult, op1=ALU.mult), reads=[x, ss, A], writes=[tmp])
        if B is None:
            return tmp
        else:
            S.op("dve", lambda e: e.tensor_tensor(out=out[:, :], in0=tmp[:, :], in1=B[:, boff:boff + D], op=ALU.add),
                 reads=[tmp, B], writes=[out])


def transpose_to(S, src, ident, pT, dst, n, copy_eng, dst_ap=None):
    for k in range(n):
        S.op("pe", lambda e, k=k: e.transpose(out=pT[:, k, :], in_=src[:, k * 128:(k + 1) * 128], identity=ident[:, :]),
             reads=[src, ident], writes=[pT])
    oap = dst_ap if dst_ap is not None else dst[:, 0:n, :]
    if copy_eng == "act":
        S.op("act", lambda e: e.copy(out=oap, in_=pT[:, 0:n, :]), reads=[pT], writes=[dst])
    else:
        S.op(copy_eng, lambda e: e.tensor_copy(out=oap, in_=pT[:, 0:n, :]), reads=[pT], writes=[dst])


def phase_mods(S, io, layer, mods_d, modc_d):
    with S.phase(f"mod{layer}"):
        jobs = [(io["c"], mods_d, layer, 12)]
        if modc_d is not None:
            jobs.append((io["c_ctx"], modc_d, 0, 4))
        wb = [S.sb(f"w{i}", [128, 8, 512], F32) for i in range(2)]
        pm = [S.ps(f"pm{i}", [128, 512], F32) for i in range(2)]
        it = 0
        for ji, (cvec, dst, drow, nch) in enumerate(jobs):
            csb = S.sb(f"c{ji}", [128, 8], F32)
            S.op("sp", lambda e, csb=csb, cvec=cvec: e.dma_start(out=csb[:, :], in_=cvec.t.rearrange("(k p) -> p k", p=128),
                                                                 allow_slow_non_contiguous=True),
                 reads=[cvec], writes=[csb], dma=True)
            sc = S.sb(f"sc{ji}", [128, 8], F32)
            S.op("act", lambda e, sc=sc, csb=csb: e.activation(out=sc[:, :], in_=csb[:, :], func=AF.Silu), reads=[csb], writes=[sc])
            rep = S.sb(f"rep{ji}", [128, 8, 128], mybir.dt.float32r if MOD_F32R else F32)
            S.op("dve", lambda e, rep=rep, sc=sc: e.tensor_copy(out=rep[:, :, :], in_=sc[:, :].unsqueeze(2).to_broadcast([128, 8, 128])),
                 reads=[sc], writes=[rep])
            n = nch * 512
            mbc = bc_load(S, f"mbc{ji}", io["b_mod"].t[layer:layer + 1, 0:n], n, io["b_mod"])
            for ch in range(nch):
                w = wb[it % 2]
                p = pm[it % 2]
                it += 1
                S.op("sp", lambda e, w=w, ch=ch: e.dma_start(
                    out=w[:, :, :], in_=io["w_mod"].t[layer, :, ch * 512:(ch + 1) * 512].rearrange("(k p) n -> p k n", p=128)),
                    reads=[io["w_mod"]], writes=[w], dma=True)
                for k in range(8):
                    if MOD_F32R:
                        S.op("pe", lambda e, k=k, w=w, p=p, rep=rep: e.matmul(p[:, :], lhsT=rep[:, k, :], rhs=w[:, k, :].bitcast(mybir.dt.float32r),
                                                                              start=(k == 0), stop=(k == 7)), reads=[rep, w], writes=[p])
                    else:
                        S.op("pe", lambda e, k=k, w=w, p=p, rep=rep: e.matmul(p[:, :], lhsT=rep[:, k, :], rhs=w[:, k, :], start=(k == 0), stop=(k == 7)),
                             reads=[rep, w], writes=[p])
                S.op("dve", lambda e, p=p, ch=ch, mbc=mbc: e.tensor_tensor(out=mbc[:, ch * 512:(ch + 1) * 512], in0=p[:, :],
                                                                          in1=mbc[:, ch * 512:(ch + 1) * 512], op=ALU.add),
                     reads=[p, mbc], writes=[mbc])
            S.op("sp", lambda e, mbc=mbc, dst=dst, drow=drow, n=n: e.dma_start(out=dst.t[drow:drow + 1, 0:n], in_=mbc[0:1, 0:n]),
                 reads=[mbc], writes=[dst], dma=True)


def load_mod_tiles(S, mods_d, row, norm_d, nrow, which):
    off = which * 3 * D
    m = bc_load(S, f"m{which}", mods_d.t[row:row + 1, off:off + 3 * D], 3 * D, mods_d)
    g = bc_load(S, f"nrm{which}", norm_d.t[nrow:nrow + 1, :], D, norm_d)
    S.op("dve", lambda e: e.scalar_tensor_tensor(out=m[:, D:2 * D], in0=m[:, D:2 * D], scalar=1.0, in1=g[:, :],
                                                 op0=ALU.add, op1=ALU.mult), reads=[m, g], writes=[m])
    return m


def phase_attn(S, io, x_in, x_out, mods_d, modc_d):
    with S.phase("att"):
        ident = make_ident(S, BF16)
        m1 = load_mod_tiles(S, mods_d, 0, io["norm_mix"], 0, 0)
        qg = bc_load(S, "qg", io["attn_q_norm"].t[0:1, :], HD, io["attn_q_norm"])
        kg = bc_load(S, "kg", io["attn_k_norm"].t[0:1, :], HD, io["attn_k_norm"])
        wqkv = S.sb("wqkv", [128, 8, 1536], BF16)
        for c3 in range(3):
            S.op("pool", lambda e, c3=c3: e.dma_start(out=wqkv[:, :, c3 * 512:(c3 + 1) * 512],
                                                      in_=io["attn_w_qkv"].t[:, c3 * 512:(c3 + 1) * 512].rearrange("(k p) n -> p k n", p=128)),
                 reads=[io["attn_w_qkv"]], writes=[wqkv], dma=True)
        KT = S.sb("KT", [128, NKV, NKT * 128], BF16)
        for g in range(NKV):
            S.op("pool", lambda e, g=g: e.memset(KT[64:128, g, :], 0.0), writes=[KT])
        VA = S.sb("VA", [128, NKT, NKV, 128], BF16)
        S.op("pool", lambda e: e.memset(VA[:, :, :, 64:128], 1.0), writes=[VA])
        nrm = Norm(S, "n")
        hbf = [S.sb("h0", [128, D], BF16)] * 2
        hT = [S.sb("hT0", [128, 8, 128], BF16)] * 2
        pT = S.ps("pT", [128, 8, 128], BF16)
        pproj = S.ps("pproj", [128, 512], F32)
        sq = S.sb("sq", [128, 512], F32)
        st = [S.sb(f"st{i}", [128, 3, 8], F32) for i in range(2)]
        qn = [S.sb(f"qn{i}", [128, 512], F32) for i in range(2)]
        r1 = S.sb("r1", [128, 512], F32)
        r2 = S.sb("r2", [128, 512], F32)
        qbf = S.sb("qbf", [128, 512], BF16)
        cs = [S.sb(f"cs{i}", [128, 2, HD], F32) for i in range(2)]
        cnt = [0]

        def load_x(t, src, n_rows_off):
            b = xb[cnt[0] % 3]
            S.op("sp", lambda e: e.dma_start(out=b[:, :], in_=src.t[n_rows_off:n_rows_off + 128, :]), reads=[src], writes=[b], dma=True)
            return b

        def make_hT(x, A, B, aoff, boff):
            i = cnt[0]
            cnt[0] += 1
            h = hbf[i % 2]
            nrm.apply(x, A, B, h, aoff, boff)
            transpose_to(S, h, ident, pT, hT[i % 2], 8, "act")
            return hT[i % 2]

        def headnorm_rope(psrc, pcol, nh, gain, rope, outbf, ocol):
            w = nh * HD
            s = st[cnt[0] % 2]
            q = qn[cnt[0] % 2]
            S.op("act", lambda e: e.activation(out=sq[:, 0:w], in_=psrc[:, pcol:pcol + w], func=AF.Square), reads=[psrc], writes=[sq])
            S.op("dve", lambda e: e.tensor_reduce(out=s[:, 0, 0:nh], in_=sq[:, 0:w].rearrange("p (h d) -> p h d", d=HD), axis=AX.X, op=ALU.add),
                 reads=[sq], writes=[s])
            S.op("act", lambda e: e.activation(out=s[:, 1, 0:nh], in_=s[:, 0, 0:nh], func=AF.Sqrt, bias=EPS, scale=1.0 / HD), reads=[s], writes=[s])
            S.op("dve", lambda e: e.reciprocal(out=s[:, 2, 0:nh], in_=s[:, 1, 0:nh]), reads=[s], writes=[s])
            S.op("dve", lambda e: e.tensor_tensor(out=q[:, 0:w].rearrange("p (h d) -> p h d", d=HD),
                                                  in0=psrc[:, pcol:pcol + w].rearrange("p (h d) -> p h d", d=HD),
                                                  in1=s[:, 2, 0:nh].unsqueeze(2).to_broadcast([128, nh, HD]), op=ALU.mult),
                 reads=[psrc, s], writes=[q])
            if rope is None:
                S.op("pool", lambda e: e.tensor_tensor(out=outbf[:, ocol:ocol + w].rearrange("p (h d) -> p h d", d=HD),
                                                       in0=q[:, 0:w].rearrange("p (h d) -> p h d", d=HD),
                                                       in1=gain[:, :].unsqueeze(1).to_broadcast([128, nh, HD]), op=ALU.mult),
                     reads=[q, gain], writes=[outbf])
                return
            S.op("pool", lambda e: e.tensor_tensor(out=q[:, 0:w].rearrange("p (h d) -> p h d", d=HD),
                                                   in0=q[:, 0:w].rearrange("p (h d) -> p h d", d=HD),
                                                   in1=gain[:, :].unsqueeze(1).to_broadcast([128, nh, HD]), op=ALU.mult),
                 reads=[q, gain], writes=[q])
            S.op("dve", lambda e: e.tensor_tensor(out=r1[:, 0:w].rearrange("p (h d) -> p h d", d=HD),
                                                  in0=q[:, 0:w].rearrange("p (h d) -> p h d", d=HD),
                                                  in1=rope[:, 0, :].unsqueeze(1).to_broadcast([128, nh, HD]), op=ALU.mult),
                 reads=[q, rope], writes=[r1])
            for sidx in range(2):
                S.op("pool", lambda e, sidx=sidx: e.tensor_tensor(
                    out=r2[:, 0:w].rearrange("p (h a s d) -> p h a s d", a=2, s=2, d=16)[:, :, :, sidx, :],
                    in0=q[:, 0:w].rearrange("p (h a s d) -> p h a s d", a=2, s=2, d=16)[:, :, :, 1 - sidx, :],
                    in1=rope[:, 1, :].rearrange("p (a s d) -> p a s d", a=2, s=2, d=16)[:, :, sidx, :].unsqueeze(1).to_broadcast([128, nh, 2, 16]),
                    op=ALU.mult), reads=[q, rope], writes=[r2])
            S.op("dve", lambda e: e.tensor_tensor(out=outbf[:, ocol:ocol + w], in0=r1[:, 0:w], in1=r2[:, 0:w], op=ALU.add),
                 reads=[r1, r2], writes=[outbf])

        def load_rope(t):
            c = cs[t % 2]
            S.op("sp", lambda e: e.dma_start(out=c[:, :, :], in_=io["rope"].t[:, t * 128:(t + 1) * 128, :].rearrange("c p d -> p c d")),
                 reads=[io["rope"]], writes=[c], dma=True)
            return c

        p1 = S.phase("att1")
        p1.__enter__()
        xb = [S.sb(f"x{i}", [128, D], F32) for i in range(3)]
        mc = bc_load(S, "mc", modc_d.t[0:1, 0:2 * D], 2 * D, modc_d)
        gmix = bc_load(S, "gmix", io["norm_mix"].t[0:1, :], D, io["norm_mix"])
        S.op("dve", lambda e: e.scalar_tensor_tensor(out=mc[:, D:2 * D], in0=mc[:, D:2 * D], scalar=1.0, in1=gmix[:, :],
                                                     op0=ALU.add, op1=ALU.mult), reads=[mc, gmix], writes=[mc])
        kbf = S.sb("kbf", [128, 256], BF16)
        pk = S.ps("pk", [64, NKV, 128], BF16)
        hbf1 = [hbf[0], S.sb("h1b", [128, D], BF16)]
        hT1 = [hT[0], S.sb("hT1b", [128, 8, 128], BF16)]
        fr = {}

        def front1(t):
            if t < NT:
                x = load_x(t, x_in, t * 128)
                A = m1
                rope = load_rope(t)
            else:
                x = load_x(t, io["ctx"], (t - NT) * 128)
                A = mc
                rope = None
            cnt[0] += 1
            h = hbf1[t % 2]
            nrm.apply(x, A, A, h, D, 0)
            transpose_to(S, h, ident, pT, hT1[t % 2], 8, "act")
            fr[t] = (hT1[t % 2], rope)

        def back1(t):
            hTt, rope = fr.pop(t)
            for k in range(8):
                S.op("pe", lambda e, k=k, hTt=hTt: e.matmul(pproj[:, :], lhsT=hTt[:, k, :], rhs=wqkv[:, k, 1024:1536], start=(k == 0), stop=(k == 7)),
                     reads=[hTt, wqkv], writes=[pproj])
            headnorm_rope(pproj, 0, NKV, kg, rope, kbf, 0)
            S.op("act", lambda e, t=t: e.copy(out=VA[:, t, :, 0:64], in_=pproj[:, 256:512].rearrange("p (g d) -> p g d", d=HD)),
                 reads=[pproj], writes=[VA])
            for g in range(NKV):
                S.op("pe", lambda e, g=g: e.transpose(out=pk[:, g, :], in_=kbf[:, g * 64:(g + 1) * 64], identity=ident[:, :]),
                     reads=[kbf, ident], writes=[pk])
            S.op("dve", lambda e, t=t: e.tensor_copy(out=KT[0:64, :, t * 128:(t + 1) * 128], in_=pk[:, :, :]), reads=[pk], writes=[KT])

        front1(0)
        for t in range(NKT):
            S.interleave((lambda t=t: front1(t + 1)) if t + 1 < NKT else None, lambda t=t: back1(t))
        p1.__exit__(None, None, None)
        p2 = S.phase("att2")
        p2.__enter__()
        wo = S.sb("wo", [128, NH, D], BF16)
        for hh in range(4):
            S.op("pool", lambda e, hh=hh: e.memset(wo[64:128, hh * 4:(hh + 1) * 4, :], 0.0), writes=[wo])
        for hh in range(2):
            S.op("pool", lambda e, hh=hh: e.dma_start(out=wo[0:64, hh * 8:(hh + 1) * 8, :],
                                                      in_=io["attn_w_o"].t[hh * 512:(hh + 1) * 512, :].rearrange("(h d) n -> d h n", d=64)),
                 reads=[io["attn_w_o"]], writes=[wo], dma=True)
        QC = 256
        NQC = T // QC
        QTs = [S.sb(f"QT{i}", [128, NH, QC], BF16) for i in range(2)]
        for i in range(2):
            S.op("pool", lambda e, i=i: e.memset(QTs[i][64:128, :, :], 0.0), writes=[QTs[i]])
        OT = S.sb("OT", [128, NH, QC], BF16)
        S.op("pool", lambda e: e.memset(OT[64:128, :, :], 0.0), writes=[OT])
        xq2 = [S.sb(f"xq{i}", [128, D], F32) for i in range(2)]
        xe = S.sb("xe", [128, D], F32)
        pS = [S.ps(f"pS{i}", [128, 4 * QC], F32) for i in range(2)]
        po = S.ps("po", [128, 4, QC], F32)
        pexp = [S.sb(f"pe{i}", [128, 4 * QC], BF16) for i in range(2)]
        rl = S.sb("rl", [64, 4, QC], F32)
        tmpy = sq
        NSTEP = NKV * NKT

        def prologue(qc):
            QT = QTs[qc % 2]
            for tt in range(2):
                t = qc * 2 + tt
                x = xq2[tt]
                S.op("sp", lambda e, x=x, t=t: e.dma_start(out=x[:, :], in_=x_in.t[t * 128:(t + 1) * 128, :]), reads=[x_in], writes=[x], dma=True)
                hTt = make_hT(x, m1, m1, D, 0)
                rope = load_rope(t)
                for half in range(2):
                    for k in range(8):
                        S.op("pe", lambda e, k=k, hTt=hTt, half=half: e.matmul(pproj[:, :], lhsT=hTt[:, k, :], rhs=wqkv[:, k, half * 512:(half + 1) * 512],
                                                                               start=(k == 0), stop=(k == 7)), reads=[hTt, wqkv], writes=[pproj])
                    headnorm_rope(pproj, 0, 8, qg, rope, qbf, 0)
                    for h8 in range(8):
                        S.op("pe", lambda e, h8=h8: e.transpose(out=pT[0:64, h8, :], in_=qbf[:, h8 * 64:(h8 + 1) * 64], identity=ident[:, :]),
                             reads=[qbf, ident], writes=[pT])
                    S.op("act", lambda e, half=half, tt=tt, QT=QT: e.copy(out=QT[0:64, half * 8:(half + 1) * 8, tt * 128:(tt + 1) * 128], in_=pT[0:64, :, :]),
                         reads=[pT], writes=[QT])

        def epilogue(qc):
            for tt in range(2):
                t = qc * 2 + tt
                xo_ = xe
                S.op("sp", lambda e, t=t: e.dma_start(out=xe[:, :], in_=x_in.t[t * 128:(t + 1) * 128, :]), reads=[x_in], writes=[xe], dma=True)
                for nch in range(2):
                    for h in range(NH):
                        S.op("pe", lambda e, h=h, nch=nch, tt=tt: e.matmul(pwo[:, :], lhsT=OTs[qc % 2][:, h, tt * 128:(tt + 1) * 128],
                                                                           rhs=wo[:, h, nch * 512:(nch + 1) * 512], start=(h == 0), stop=(h == NH - 1)),
                             reads=[OTs[qc % 2], wo], writes=[pwo])
                    S.op("dve", lambda e, nch=nch: e.tensor_tensor(out=tmpy[:, :], in0=pwo[:, :], in1=m1[:, 2 * D + nch * 512:2 * D + (nch + 1) * 512], op=ALU.mult),
                         reads=[pwo, m1], writes=[tmpy])
                    S.op("pool", lambda e, nch=nch, xo_=xo_: e.tensor_tensor(out=xo_[:, nch * 512:(nch + 1) * 512], in0=tmpy[:, :],
                                                                           in1=xo_[:, nch * 512:(nch + 1) * 512], op=ALU.add),
                         reads=[tmpy, xo_], writes=[xo_])
                S.op("pool", lambda e, t=t, xo_=xo_: e.dma_start(out=x_out.t[t * 128:(t + 1) * 128, :], in_=xo_[:, :]), reads=[xo_], writes=[x_out], dma=True)

        OTs = [OT, OT]
        pwo = pproj

        def emit_qk(qc, idx):
            g, kt = divmod(idx, NKT)
            ps_ = pS[idx % 2]
            QT = QTs[qc % 2]
            for j in range(2):
                S.op("pe", lambda e, j=j, g=g, kt=kt, ps_=ps_, QT=QT: e.matmul(ps_[:, j * 2 * QC:(j + 1) * 2 * QC], lhsT=KT[:, g, kt * 128:(kt + 1) * 128],
                                                                             rhs=QT[:, g * 4 + 2 * j:g * 4 + 2 * j + 2, :], start=True, stop=True),
                     reads=[KT, QT], writes=[ps_])

        prologue(0)
        for qc in range(NQC):
            pend = []
            S.defer = pend
            if qc >= 1:
                epilogue(qc - 1)
            if qc + 1 < NQC:
                prologue(qc + 1)
            S.defer = None
            OTc = OTs[qc % 2]
            emit_qk(qc, 0)
            for idx in range(NSTEP):
                g, kt = divmod(idx, NKT)
                ps_ = pS[idx % 2]
                px = pexp[idx % 2]
                if idx + 1 < NSTEP:
                    emit_qk(qc, idx + 1)
                S.op("act", lambda e, ps_=ps_, px=px: e.activation(out=px[:, :], in_=ps_[:, :], func=AF.Exp, scale=SCALE), reads=[ps_], writes=[px])
                for j in range(2):
                    S.op("pe", lambda e, j=j, g=g, kt=kt, px=px: e.matmul(po[:, 2 * j:2 * j + 2, :], lhsT=VA[:, kt, g, :], rhs=px[:, j * 2 * QC:(j + 1) * 2 * QC],
                                                                        start=(kt == 0), stop=(kt == NKT - 1)),
                         reads=[VA, px], writes=[po])
                if kt == NKT - 1:
                    S.op("dve", lambda e: e.reciprocal(out=rl[:, :, :], in_=po[64:128, :, :]), reads=[po], writes=[rl])
                    S.op("dve", lambda e, g=g, OTc=OTc: e.tensor_tensor(out=OTc[0:64, g * 4:(g + 1) * 4, :], in0=po[0:64, :, :], in1=rl[:, :, :], op=ALU.mult),
                         reads=[po, rl], writes=[OTc])
                S.replay(pend, 3 if idx < 28 else 2)
            S.replay(pend, len(pend))
        epilogue(NQC - 1)
        p2.__exit__(None, None, None)


def phase_moe(S, io, layer, x_in, x_out, mods_d, final_norm=False):
    HT = 16
    for half in range(2):
        with S.phase(f"moe{layer}{half}"):
            identf = make_ident(S, F32)
            identb = S.sb("idb", [128, 128], BF16)
            S.op("dve", lambda e: e.tensor_copy(out=identb[:, :], in_=identf[:, :]), reads=[identf], writes=[identb])
            m2 = load_mod_tiles(S, mods_d, layer, io["norm_ffn"], layer, 1)
            wr = S.sb("wr", [128, 8, 36], F32)
            S.op("sp", lambda e: e.dma_start(out=wr[:, :, 0:4], in_=io["moe_w_rg"].t[layer].rearrange("(k p) n -> p k n", p=128), allow_slow_non_contiguous=True),
                 reads=[io["moe_w_rg"]], writes=[wr], dma=True)
            S.op("sp", lambda e: e.dma_start(out=wr[:, :, 4:36], in_=io["moe_w_re"].t[layer].rearrange("(k p) n -> p k n", p=128), allow_slow_non_contiguous=True),
                 reads=[io["moe_w_re"]], writes=[wr], dma=True)
            br = S.sb("br", [128, 36], F32)
            S.op("sp", lambda e: e.dma_start(out=br[:, 0:4], in_=io["moe_b_rg"].t[layer:layer + 1, :].to_broadcast([128, 4])), reads=[io["moe_b_rg"]], writes=[br], dma=True)
            S.op("sp", lambda e: e.dma_start(out=br[:, 4:36], in_=io["moe_b_re"].t[layer:layer + 1, :].to_broadcast([128, 32])), reads=[io["moe_b_re"]], writes=[br], dma=True)
            nrm = Norm(S, "n")
            hTall = S.sb("hTall", [128, 8, HT * 128], BF16)
            G = S.sb("G", [128, HT, NE], F32)
            acc = S.sb("acc", [128, HT, D], F32)
            xb = [S.sb(f"x{i}", [128, D], F32) for i in range(2)]
            h2 = [S.sb(f"h2{i}", [128, D], F32) for i in range(2)]
            pTf = S.ps("pTf", [128, 8, 128], F32)
            hTf = S.sb("hTf", [128, 8, 128], F32)
            pl = S.ps("pl", [128, 512], F32)
            L = S.sb("L", [128, 36], F32)
            rt = S.sb("rt", [128, 16], F32)
            mg = S.sb("mg", [128, 4], F32)
            pen = S.sb("pen", [128, 4], F32)
            eg = S.sb("eg", [128, 4], F32)
            lem = S.sb("lem", [128, 32], F32)
            mx8 = S.sb("mx8", [128, 8], F32)
            sel = S.sb("sel", [128, 32], F32)
            ez = S.sb("ez", [128, 32], F32)
            MDBG = os.environ.get("MOE_DBG", "")
            if MDBG == "p2":
                S.op("pool", lambda e: e.memset(G[:, :, :], 1.0 / 32), writes=[G])
            for tt in range(HT):
                t = half * HT + tt
                x = xb[tt % 2]
                h = h2[tt % 2]
                if MDBG == "p1a0":
                    continue
                S.op("sp", lambda e, x=x, t=t: e.dma_start(out=x[:, :], in_=x_in.t[t * 128:(t + 1) * 128, :]), reads=[x_in], writes=[x], dma=True)
                nrm.apply(x, m2, m2, h, D, 0)
                if MDBG == "p1a1":
                    continue
                for k in range(8):
                    S.op("pe", lambda e, k=k, h=h: e.transpose(out=pTf[:, k, :], in_=h[:, k * 128:(k + 1) * 128], identity=identf[:, :]),
                         reads=[h, identf], writes=[pTf])
                S.op("act", lambda e: e.copy(out=hTf[:, :, :], in_=pTf[:, :, :]), reads=[pTf], writes=[hTf])
                if MDBG == "p1a2":
                    continue
                S.op("pool", lambda e, tt=tt: e.tensor_copy(out=hTall[:, :, tt * 128:(tt + 1) * 128], in_=hTf[:, :, :]), reads=[hTf], writes=[hTall])
                if MDBG in ("p2", "p1a", "p1a0", "p1a1", "p1a2"):
                    continue
                for k in range(8):
                    S.op("pe", lambda e, k=k: e.matmul(pl[:, 0:36], lhsT=hTf[:, k, :], rhs=wr[:, k, :], start=(k == 0), stop=(k == 7)),
                         reads=[hTf, wr], writes=[pl])
                S.op("dve", lambda e: e.tensor_tensor(out=L[:, :], in0=pl[:, 0:36], in1=br[:, :], op=ALU.add), reads=[pl, br], writes=[L])
                if MDBG == "p1b":
                    continue
                S.op("dve", lambda e: e.tensor_reduce(out=rt[:, 0:1], in_=L[:, 0:4], axis=AX.X, op=ALU.max), reads=[L], writes=[rt])
                S.op("dve", lambda e: e.tensor_scalar(out=mg[:, :], in0=L[:, 0:4], scalar1=rt[:, 0:1], scalar2=None, op0=ALU.is_ge), reads=[L, rt], writes=[mg])
                S.op("dve", lambda e: e.tensor_scalar(out=rt[:, 1:2], in0=rt[:, 0:1], scalar1=-1.0, scalar2=None, op0=ALU.mult), reads=[rt], writes=[rt])
                S.op("pool", lambda e: e.memset(rt[:, 2:3], 0.0), writes=[rt])
                S.op("act", lambda e: e.activation(out=eg[:, :], in_=L[:, 0:4], func=AF.Exp, bias=rt[:, 1:2], scale=1.0, accum_out=rt[:, 2:3]),
                     reads=[L, rt], writes=[eg, rt])
                S.op("dve", lambda e: e.reciprocal(out=rt[:, 3:4], in_=rt[:, 2:3]), reads=[rt], writes=[rt])
                S.op("dve", lambda e: e.tensor_scalar(out=pen[:, :], in0=mg[:, :], scalar1=-1.0, scalar2=1e30, op0=ALU.add, op1=ALU.mult), reads=[mg], writes=[pen])
                S.op("dve", lambda e: e.tensor_tensor(out=lem[:, :].rearrange("p (g j) -> p g j", j=8), in0=L[:, 4:36].rearrange("p (g j) -> p g j", j=8),
                                                      in1=pen[:, :].unsqueeze(2).to_broadcast([128, 4, 8]), op=ALU.add), reads=[L, pen], writes=[lem])
                S.op("dve", lambda e: e.max(out=mx8[:, :], in_=lem[:, :]), reads=[lem], writes=[mx8])
                S.op("dve", lambda e: e.tensor_scalar(out=sel[:, :], in0=lem[:, :], scalar1=mx8[:, 1:2], scalar2=None, op0=ALU.is_ge), reads=[lem, mx8], writes=[sel])
                S.op("dve", lambda e: e.tensor_scalar(out=rt[:, 4:5], in0=mx8[:, 0:1], scalar1=-1.0, scalar2=None, op0=ALU.mult), reads=[mx8], writes=[rt])
                S.op("act", lambda e: e.activation(out=ez[:, :], in_=lem[:, :], func=AF.Exp, bias=rt[:, 4:5], scale=1.0), reads=[lem, rt], writes=[ez])
                S.op("dve", lambda e: e.tensor_tensor(out=ez[:, :], in0=ez[:, :], in1=sel[:, :], op=ALU.mult), reads=[ez, sel], writes=[ez])
                S.op("dve", lambda e: e.tensor_reduce(out=rt[:, 5:6], in_=ez[:, :], axis=AX.X, op=ALU.add), reads=[ez], writes=[rt])
                S.op("dve", lambda e: e.reciprocal(out=rt[:, 6:7], in_=rt[:, 5:6]), reads=[rt], writes=[rt])
                S.op("dve", lambda e: e.tensor_tensor(out=rt[:, 7:8], in0=rt[:, 6:7], in1=rt[:, 3:4], op=ALU.mult), reads=[rt], writes=[rt])
                S.op("dve", lambda e, tt=tt: e.tensor_scalar(out=G[:, tt, :], in0=ez[:, :], scalar1=rt[:, 7:8], scalar2=None, op0=ALU.mult), reads=[ez, rt], writes=[G])
            w1b = [S.sb(f"w1_{i}", [128, 8, DE], BF16) for i in range(2)]
            w3b = [S.sb(f"w3_{i}", [128, 8, DE], BF16) for i in range(2)]
            w2b = [S.sb(f"w2_{i}", [128, 4, D], BF16) for i in range(2)]
            ph1 = S.ps("ph1", [128, DE], F32)
            ph3 = S.ps("ph3", [128, DE], F32)
            pa = S.ps("pa", [128, 8, 128], BF16)
            py = [S.ps(f"py{i}", [128, 512], F32) for i in range(2)]
            s1 = [S.sb(f"s1_{i}", [128, DE], F32) for i in range(2)]
            ab = [S.sb(f"ab{i}", [128, DE], BF16) for i in range(2)]
            aT = [S.sb(f"aT{i}", [128, 4, 128], BF16) for i in range(2)]
            for tt in range(HT):
                S.op("pool", lambda e, tt=tt: e.memset(acc[:, tt, :], 0.0), writes=[acc])
            it = 0
            for ex in range(NE if not MDBG.startswith("p1") else 0):
                w1, w3, w2 = w1b[ex % 2], w3b[ex % 2], w2b[ex % 2]
                S.op("pool", lambda e, w1=w1, ex=ex: e.dma_start(out=w1[:, :, :], in_=io["moe_w1"].t[layer, ex].rearrange("(k p) n -> p k n", p=128)),
                     reads=[io["moe_w1"]], writes=[w1], dma=True)
                S.op("pool", lambda e, w3=w3, ex=ex: e.dma_start(out=w3[:, :, :], in_=io["moe_w3"].t[layer, ex].rearrange("(k p) n -> p k n", p=128)),
                     reads=[io["moe_w3"]], writes=[w3], dma=True)
                for hh in range(2):
                    S.op("pool", lambda e, w2=w2, ex=ex, hh=hh: e.dma_start(out=w2[:, :, hh * 512:(hh + 1) * 512],
                                                                            in_=io["moe_w2"].t[layer, ex, :, hh * 512:(hh + 1) * 512].rearrange("(k p) n -> p k n", p=128)),
                         reads=[io["moe_w2"]], writes=[w2], dma=True)
                for tt in range(HT):
                    i2 = it % 2
                    it += 1
                    for k in range(8):
                        S.op("pe", lambda e, k=k, tt=tt, w1=w1: e.matmul(ph1[:, :], lhsT=hTall[:, k, tt * 128:(tt + 1) * 128], rhs=w1[:, k, :], start=(k == 0), stop=(k == 7)),
                             reads=[hTall, w1], writes=[ph1])
                    for k in range(8):
                        S.op("pe", lambda e, k=k, tt=tt, w3=w3: e.matmul(ph3[:, :], lhsT=hTall[:, k, tt * 128:(tt + 1) * 128], rhs=w3[:, k, :], start=(k == 0), stop=(k == 7)),
                             reads=[hTall, w3], writes=[ph3])
                    S.op("act", lambda e, i2=i2: e.activation(out=s1[i2][:, :], in_=ph1[:, :], func=AF.Silu), reads=[ph1], writes=[s1[i2]])
                    S.op("dve", lambda e, i2=i2: e.tensor_tensor(out=ab[i2][:, :], in0=ph3[:, :], in1=s1[i2][:, :], op=ALU.mult), reads=[ph3, s1[i2]], writes=[ab[i2]])
                    for fc in range(4):
                        S.op("pe", lambda e, fc=fc, i2=i2: e.transpose(out=pa[:, fc, :], in_=ab[i2][:, fc * 128:(fc + 1) * 128], identity=identb[:, :]),
                             reads=[ab[i2], identb], writes=[pa])
                    S.op("act", lambda e, i2=i2: e.copy(out=aT[i2][:, :, :], in_=pa[:, 0:4, :]), reads=[pa], writes=[aT[i2]])
                    for nch in range(2):
                        for fc in range(4):
                            S.op("pe", lambda e, fc=fc, nch=nch, i2=i2, w2=w2: e.matmul(py[nch][:, :], lhsT=aT[i2][:, fc, :], rhs=w2[:, fc, nch * 512:(nch + 1) * 512],
                                                                                       start=(fc == 0), stop=(fc == 3)), reads=[aT[i2], w2], writes=[py[nch]])
                        S.op("dve", lambda e, nch=nch, tt=tt, ex=ex: e.scalar_tensor_tensor(out=acc[:, tt, nch * 512:(nch + 1) * 512], in0=py[nch][:, :],
                                                                                            scalar=G[:, tt, ex:ex + 1], in1=acc[:, tt, nch * 512:(nch + 1) * 512],
                                                                                            op0=ALU.mult, op1=ALU.add), reads=[py[nch], G, acc], writes=[acc])
            if final_norm:
                fn = bc_load(S, "fn", io["final_norm"].t.rearrange("(o n) -> o n", o=1), D, io["final_norm"])
            for tt in range(HT):
                t = half * HT + tt
                x = xb[tt % 2]
                o = h2[tt % 2]
                S.op("sp", lambda e, x=x, t=t: e.dma_start(out=x[:, :], in_=x_in.t[t * 128:(t + 1) * 128, :]), reads=[x_in], writes=[x], dma=True)
                S.op("dve", lambda e, tt=tt, o=o: e.tensor_tensor(out=o[:, :], in0=acc[:, tt, :], in1=m2[:, 2 * D:3 * D], op=ALU.mult), reads=[acc, m2], writes=[o])
                S.op("pool", lambda e, o=o, x=x: e.tensor_tensor(out=o[:, :], in0=o[:, :], in1=x[:, :], op=ALU.add), reads=[o, x], writes=[o])
                if final_norm:
                    o = nrm.apply(o, fn, None, None, 0, 0)
                S.op("sp", lambda e, t=t, o=o: e.dma_start(out=x_out.t[t * 128:(t + 1) * 128, :], in_=o[:, :]), reads=[o], writes=[x_out], dma=True)


SLOT = 256
NBLK = 64


def phase_moe_sorted(S, io, layer, x_in, x_out, mods_d, scr, final_norm=False):
    h2_d, xs_d, yb_d = scr
    with S.phase(f"ms{layer}"):
        IDXi = S.sb("IDXi", [128, NT, 2], I32)
        GAB = S.sb("GAB", [128, NT, 2], F32)
        WIDXi = S.sb("WIDXi", [128, NBLK], I32)
        m2 = load_mod_tiles(S, mods_d, layer, io["norm_ffn"], layer, 1)
        pA = S.phase(f"msA{layer}")
        pA.__enter__()
        identf = make_ident(S, F32)
        wr = S.sb("wr", [128, 8, 36], F32)
        S.op("sp", lambda e: e.dma_start(out=wr[:, :, 0:4], in_=io["moe_w_rg"].t[layer].rearrange("(k p) n -> p k n", p=128), allow_slow_non_contiguous=True),
             reads=[io["moe_w_rg"]], writes=[wr], dma=True)
        S.op("sp", lambda e: e.dma_start(out=wr[:, :, 4:36], in_=io["moe_w_re"].t[layer].rearrange("(k p) n -> p k n", p=128), allow_slow_non_contiguous=True),
             reads=[io["moe_w_re"]], writes=[wr], dma=True)
        br = S.sb("br", [128, 36], F32)
        S.op("sp", lambda e: e.dma_start(out=br[:, 0:4], in_=io["moe_b_rg"].t[layer:layer + 1, :].to_broadcast([128, 4])), reads=[io["moe_b_rg"]], writes=[br], dma=True)
        S.op("sp", lambda e: e.dma_start(out=br[:, 4:36], in_=io["moe_b_re"].t[layer:layer + 1, :].to_broadcast([128, 32])), reads=[io["moe_b_re"]], writes=[br], dma=True)
        trif = S.sb("trif", [128, 128], F32)
        dma(S, "sp", trif, trif[:, :], io["tri"], io["tri"].t[:, :])
        trib = S.sb("trib", [128, 128], BF16)
        S.op("dve", lambda e: e.tensor_copy(out=trib[:, :], in_=trif[:, :]), reads=[trif], writes=[trib])
        onesb = S.sb("onesb", [128, 128], BF16)
        S.op("pool", lambda e: e.memset(onesb[:, :], 1.0), writes=[onesb])
        thr = bc_load(S, "thr", io["thr"].t[0:1, :], 16 + NBLK, io["thr"])
        pidx = S.sb("pidx", [128, 1], F32)
        dma(S, "sp", pidx, pidx[:, :], io["pidx"], io["pidx"].t[:, :])
        zt = S.sb("zt", [128, 8 * D], BF16)
        S.op("pool", lambda e: e.memset(zt[:, :], 0.0), writes=[zt])
        for z in range(NBLK * SLOT // 128 // 8):
            S.op("sp", lambda e, z=z: e.dma_start(out=xs_d.t.rearrange("(p r) n -> p (r n)", p=128)[:, z * 8 * D:(z + 1) * 8 * D], in_=zt[:, :]),
                 reads=[zt], writes=[xs_d], dma=True)
        nrm = Norm(S, "n")
        SEL = S.sb("SEL", [128, NT, NE], F32)
        G = S.sb("G", [128, NT, NE], F32)
        RANK = S.sb("RANK", [128, NT, NE], F32)
        tot = S.sb("tot", [128, NE], F32)
        S.op("pool", lambda e: e.memset(tot[:, :], 0.0), writes=[tot])
        xb = [S.sb(f"x{i}", [128, D], F32) for i in range(2)]
        h2 = [S.sb(f"h2{i}", [128, D], F32) for i in range(2)]
        h2b = [S.sb(f"h2b{i}", [128, D], BF16) for i in range(2)]
        pTf = S.ps("pTf", [128, 8, 128], F32)
        hTf = S.sb("hTf", [128, 8, 128], F32)
        pl = S.ps("pl", [128, 512], F32)
        prk = S.ps("prk", [128, 512], F32)
        L = S.sb("L", [128, 36], F32)
        rt = S.sb("rt", [128, 16], F32)
        mg = S.sb("mg", [128, 4], F32)
        pen = S.sb("pen", [128, 4], F32)
        eg = S.sb("eg", [128, 4], F32)
        lem = S.sb("lem", [128, 32], F32)
        mx8 = S.sb("mx8", [128, 8], F32)
        ez = S.sb("ez", [128, 32], F32)
        selb = S.sb("selb", [128, 32], BF16)
        pTf2 = S.ps("pTf2", [128, 8, 128], F32)
        pl2 = S.ps("pl2", [128, 512], F32)
        hTf2 = S.sb("hTf2", [128, 8, 128], F32)
        L2 = S.sb("L2", [128, 36], F32)
        pTfs, pls, hTfs, Ls = [pTf, pTf2], [pl, pl2], [hTf, hTf2], [L, L2]

        def front(tt):
            x = xb[tt % 2]
            h = h2[tt % 2]
            hb = h2b[tt % 2]
            pTf_, pl_, hTf_, L_ = pTfs[tt % 2], pls[tt % 2], hTfs[tt % 2], Ls[tt % 2]
            S.op("sp", lambda e, x=x, tt=tt: e.dma_start(out=x[:, :], in_=x_in.t[tt * 128:(tt + 1) * 128, :]), reads=[x_in], writes=[x], dma=True)
            nrm.apply(x, m2, m2, h, D, 0)
            S.op("pool", lambda e, h=h, hb=hb: e.tensor_copy(out=hb[:, :], in_=h[:, :]), reads=[h], writes=[hb])
            S.op("pool", lambda e, hb=hb, tt=tt: e.dma_start(out=h2_d.t[tt * 128:(tt + 1) * 128, :], in_=hb[:, :]), reads=[hb], writes=[h2_d], dma=True)
            for k in range(8):
                S.op("pe", lambda e, k=k, h=h: e.transpose(out=pTf_[:, k, :], in_=h[:, k * 128:(k + 1) * 128], identity=identf[:, :]),
                     reads=[h, identf], writes=[pTf_])
            S.op("act", lambda e: e.copy(out=hTf_[:, :, :], in_=pTf_[:, :, :]), reads=[pTf_], writes=[hTf_])
            for k in range(8):
                S.op("pe", lambda e, k=k: e.matmul(pl_[:, 0:36], lhsT=hTf_[:, k, :], rhs=wr[:, k, :], start=(k == 0), stop=(k == 7)),
                     reads=[hTf_, wr], writes=[pl_])
            S.op("dve", lambda e: e.tensor_tensor(out=L_[:, :], in0=pl_[:, 0:36], in1=br[:, :], op=ALU.add), reads=[pl_, br], writes=[L_])

        def back(tt, L):
            S.op("dve", lambda e: e.tensor_reduce(out=rt[:, 0:1], in_=L[:, 0:4], axis=AX.X, op=ALU.max), reads=[L], writes=[rt])
            S.op("dve", lambda e: e.tensor_scalar(out=mg[:, :], in0=L[:, 0:4], scalar1=rt[:, 0:1], scalar2=None, op0=ALU.is_ge), reads=[L, rt], writes=[mg])
            S.op("dve", lambda e: e.tensor_scalar(out=rt[:, 1:2], in0=rt[:, 0:1], scalar1=-1.0, scalar2=None, op0=ALU.mult), reads=[rt], writes=[rt])
            S.op("pool", lambda e: e.memset(rt[:, 2:3], 0.0), writes=[rt])
            S.op("act", lambda e: e.activation(out=eg[:, :], in_=L[:, 0:4], func=AF.Exp, bias=rt[:, 1:2], scale=1.0, accum_out=rt[:, 2:3]),
                 reads=[L, rt], writes=[eg, rt])
            S.op("dve", lambda e: e.reciprocal(out=rt[:, 3:4], in_=rt[:, 2:3]), reads=[rt], writes=[rt])
            S.op("dve", lambda e: e.tensor_scalar(out=pen[:, :], in0=mg[:, :], scalar1=-1.0, scalar2=1e30, op0=ALU.add, op1=ALU.mult), reads=[mg], writes=[pen])
            S.op("dve", lambda e: e.tensor_tensor(out=lem[:, :].rearrange("p (g j) -> p g j", j=8), in0=L[:, 4:36].rearrange("p (g j) -> p g j", j=8),
                                                  in1=pen[:, :].unsqueeze(2).to_broadcast([128, 4, 8]), op=ALU.add), reads=[L, pen], writes=[lem])
            S.op("dve", lambda e: e.max(out=mx8[:, :], in_=lem[:, :]), reads=[lem], writes=[mx8])
            S.op("dve", lambda e, tt=tt: e.tensor_scalar(out=SEL[:, tt, :], in0=lem[:, :], scalar1=mx8[:, 1:2], scalar2=None, op0=ALU.is_ge), reads=[lem, mx8], writes=[SEL])
            S.op("dve", lambda e: e.tensor_scalar(out=rt[:, 4:5], in0=mx8[:, 0:1], scalar1=-1.0, scalar2=None, op0=ALU.mult), reads=[mx8], writes=[rt])
            S.op("act", lambda e: e.activation(out=ez[:, :], in_=lem[:, :], func=AF.Exp, bias=rt[:, 4:5], scale=1.0), reads=[lem, rt], writes=[ez])
            S.op("dve", lambda e, tt=tt: e.tensor_tensor(out=ez[:, :], in0=ez[:, :], in1=SEL[:, tt, :], op=ALU.mult), reads=[ez, SEL], writes=[ez])
            S.op("dve", lambda e: e.tensor_reduce(out=rt[:, 5:6], in_=ez[:, :], axis=AX.X, op=ALU.add), reads=[ez], writes=[rt])
            S.op("dve", lambda e: e.reciprocal(out=rt[:, 6:7], in_=rt[:, 5:6]), reads=[rt], writes=[rt])
            S.op("dve", lambda e: e.tensor_tensor(out=rt[:, 7:8], in0=rt[:, 6:7], in1=rt[:, 3:4], op=ALU.mult), reads=[rt], writes=[rt])
            S.op("dve", lambda e, tt=tt: e.tensor_scalar(out=G[:, tt, :], in0=ez[:, :], scalar1=rt[:, 7:8], scalar2=None, op0=ALU.mult), reads=[ez, rt], writes=[G])
            S.op("pool", lambda e, tt=tt: e.tensor_copy(out=selb[:, :], in_=SEL[:, tt, :]), reads=[SEL], writes=[selb])
            S.op("pe", lambda e: e.matmul(prk[:, 0:32], lhsT=trib[:, :], rhs=selb[:, :], start=True, stop=True), reads=[trib, selb], writes=[prk])
            S.op("pe", lambda e: e.matmul(prk[:, 32:64], lhsT=onesb[:, :], rhs=selb[:, :], start=True, stop=True), reads=[onesb, selb], writes=[prk])
            S.op("dve", lambda e, tt=tt: e.tensor_tensor(out=RANK[:, tt, :], in0=prk[:, 0:32], in1=tot[:, :], op=ALU.add), reads=[prk, tot], writes=[RANK])
            S.op("dve", lambda e: e.tensor_tensor(out=tot[:, :], in0=prk[:, 32:64], in1=tot[:, :], op=ALU.add), reads=[prk, tot], writes=[tot])

        front(0)
        for tt in range(NT):
            S.interleave((lambda tt=tt: front(tt + 1)) if tt + 1 < NT else None, lambda tt=tt: back(tt, Ls[tt % 2]))
        cmp3 = S.sb("cmp3", [128, NE, 16], F32)
        nb = S.sb("nb", [128, NE], F32)
        sa = S.sb("sa", [128, NE], F32)
        sb_ = S.sb("sb_", [128, NE], F32)
        pc = S.sb("pc", [128, NE], F32)
        start = S.sb("start", [128, NE], F32)
        S.op("dve", lambda e: e.tensor_tensor(out=cmp3[:, :, :], in0=tot[:, :].unsqueeze(2).to_broadcast([128, NE, 16]),
                                              in1=thr[:, 0:16].unsqueeze(1).to_broadcast([128, NE, 16]), op=ALU.is_gt), reads=[tot, thr], writes=[cmp3])
        S.op("dve", lambda e: e.tensor_reduce(out=nb[:, :], in_=cmp3[:, :, :], axis=AX.X, op=ALU.add), reads=[cmp3], writes=[nb])
        S.op("dve", lambda e: e.tensor_scalar(out=pc[:, :], in0=nb[:, :], scalar1=float(SLOT), scalar2=None, op0=ALU.mult), reads=[nb], writes=[pc])
        S.op("dve", lambda e: e.tensor_copy(out=sa[:, :], in_=pc[:, :]), reads=[pc], writes=[sa])
        a, b = sa, sb_
        for sft in (1, 2, 4, 8, 16):
            S.op("dve", lambda e, a=a, b=b, sft=sft: e.tensor_tensor(out=b[:, sft:NE], in0=a[:, sft:NE], in1=a[:, 0:NE - sft], op=ALU.add), reads=[a], writes=[b])
            S.op("dve", lambda e, a=a, b=b, sft=sft: e.tensor_copy(out=b[:, 0:sft], in_=a[:, 0:sft]), reads=[a], writes=[b])
            a, b = b, a
        incl = a
        S.op("dve", lambda e: e.tensor_tensor(out=start[:, :], in0=incl[:, :], in1=pc[:, :], op=ALU.subtract), reads=[incl, pc], writes=[start])
        cmpj = S.sb("cmpj", [128, NBLK, NE], F32)
        ej = S.sb("ej", [128, NBLK], F32)
        S.op("dve", lambda e: e.tensor_tensor(out=cmpj[:, :, :], in0=incl[:, :].unsqueeze(1).to_broadcast([128, NBLK, NE]),
                                              in1=thr[:, 16:16 + NBLK].unsqueeze(2).to_broadcast([128, NBLK, NE]), op=ALU.is_le), reads=[incl, thr], writes=[cmpj])
        ejr = S.sb("ejr", [128, NBLK], F32)
        ejm = S.sb("ejm", [128, NBLK], F32)
        S.op("dve", lambda e: e.tensor_reduce(out=ejr[:, :], in_=cmpj[:, :, :], axis=AX.X, op=ALU.add), reads=[cmpj], writes=[ejr])
        S.op("dve", lambda e: e.tensor_copy(out=ej[:, :], in_=ejr[:, :]), reads=[ejr], writes=[ej])
        S.op("dve", lambda e: e.tensor_scalar(out=ej[:, :], in0=ej[:, :], scalar1=128.0, scalar2=pidx[:, 0:1], op0=ALU.mult, op1=ALU.add), reads=[ej, pidx], writes=[ej])
        S.op("dve", lambda e: e.tensor_scalar(out=ejm[:, :], in0=ejr[:, :], scalar1=float(NE) - 0.5, scalar2=1.0e6, op0=ALU.is_gt, op1=ALU.mult), reads=[ejr], writes=[ejm])
        ejs = S.sb("ejs", [128, NBLK], F32)
        S.op("dve", lambda e: e.memset(ejs[:, 0:1], 0.0), writes=[ejs])
        S.op("dve", lambda e: e.tensor_tensor(out=ejs[:, 1:NBLK], in0=ejr[:, 1:NBLK], in1=ejr[:, 0:NBLK - 1], op=ALU.is_equal), reads=[ejr], writes=[ejs])
        S.op("dve", lambda e: e.scalar_tensor_tensor(out=ejm[:, :], in0=ejs[:, :], scalar=1.0e6, in1=ejm[:, :], op0=ALU.mult, op1=ALU.add), reads=[ejs, ejm], writes=[ejm])
        S.op("dve", lambda e: e.scalar_tensor_tensor(out=ej[:, :], in0=ej[:, :], scalar=float(layer * NE * 128), in1=ejm[:, :], op0=ALU.add, op1=ALU.add),
             reads=[ej, ejm], writes=[ej])
        S.op("dve", lambda e: e.tensor_copy(out=WIDXi[:, :], in_=ej[:, :]), reads=[ej], writes=[WIDXi])
        md = S.sb("md", [128, NE], F32)
        eq = S.sb("eq", [128, NE], F32)
        idxf = S.sb("idxf", [128, NT, 2], F32)
        for tt in range(NT):
            S.op("dve", lambda e, tt=tt: e.tensor_tensor(out=md[:, :], in0=RANK[:, tt, :], in1=start[:, :], op=ALU.add), reads=[RANK, start], writes=[md])
            S.op("dve", lambda e, tt=tt: e.scalar_tensor_tensor(out=md[:, :], in0=md[:, :], scalar=1.0, in1=SEL[:, tt, :], op0=ALU.add, op1=ALU.mult),
                 reads=[md, SEL], writes=[md])
            S.op("dve", lambda e: e.max(out=mx8[:, :], in_=md[:, :]), reads=[md], writes=[mx8])
            S.op("dve", lambda e, tt=tt: e.tensor_scalar(out=idxf[:, tt, :], in0=mx8[:, 0:2], scalar1=-1.0, scalar2=None, op0=ALU.add), reads=[mx8], writes=[idxf])
            for ab in range(2):
                S.op("dve", lambda e, ab=ab: e.tensor_scalar(out=eq[:, :], in0=md[:, :], scalar1=mx8[:, ab:ab + 1], scalar2=None, op0=ALU.is_equal), reads=[md, mx8], writes=[eq])
                S.op("dve", lambda e, tt=tt: e.tensor_tensor(out=eq[:, :], in0=eq[:, :], in1=G[:, tt, :], op=ALU.mult), reads=[eq, G], writes=[eq])
                S.op("dve", lambda e, tt=tt, ab=ab: e.tensor_reduce(out=GAB[:, tt, ab:ab + 1], in_=eq[:, :], axis=AX.X, op=ALU.add), reads=[eq], writes=[GAB])
        S.op("dve", lambda e: e.tensor_copy(out=IDXi[:, :, :], in_=idxf[:, :, :]), reads=[idxf], writes=[IDXi])
        pA.__exit__(None, None, None)
        pB = S.phase(f"msB{layer}")
        pB.__enter__()
        hb2 = [S.sb(f"hb{i}", [128, D], BF16) for i in range(3)]
        for tt in range(NT):
            hb = hb2[tt % 3]
            S.op("sp", lambda e, hb=hb, tt=tt: e.dma_start(out=hb[:, :], in_=h2_d.t[tt * 128:(tt + 1) * 128, :]), reads=[h2_d], writes=[hb], dma=True)
            for ab in range(2):
                S.op("pool", lambda e, hb=hb, tt=tt, ab=ab: e.indirect_dma_start(out=xs_d.t[:, :], out_offset=bass.IndirectOffsetOnAxis(ap=IDXi[:, tt, ab:ab + 1], axis=0),
                                                                               in_=hb[:, :], in_offset=None),
                     reads=[hb, IDXi], writes=[xs_d], dma=True)
        pB.__exit__(None, None, None)
        pC = S.phase(f"msC{layer}")
        pC.__enter__()
        if getattr(S, "bc_reg", None) is None:
            S.bc_reg = S.gstack.enter_context(S.nc.gpsimd.register("bcreg"))
            S.op("pool", lambda e: e.reg_mov(S.bc_reg, 2 * NE * 128 - 1))
        identb = make_ident(S, BF16)
        w1s = [S.sb("w1s0", [128, 8 * DE], F32)] * 2
        w3s = [S.sb("w3s0", [128, 8 * DE], F32)] * 2
        w2s = [S.sb("w2s0", [128, 4 * D], F32)] * 2
        w1c = [S.sb(f"w1c{i}", [128, 8 * DE], BF16) for i in range(2)]
        w3c = [S.sb(f"w3c{i}", [128, 8 * DE], BF16) for i in range(2)]
        w2c = [S.sb(f"w2c{i}", [128, 4 * D], BF16) for i in range(2)]
        w1rows = io["moe_w1"].t.rearrange("l e (p r) n -> (l e p) (r n)", r=8)
        w3rows = io["moe_w3"].t.rearrange("l e (p r) n -> (l e p) (r n)", r=8)
        w2rows = io["moe_w2"].t.rearrange("l e (p r) n -> (l e p) (r n)", r=4)
        xs = [S.sb(f"xs{i}", [128, D], BF16) for i in range(2)]
        xT = [S.sb(f"xT{i}", [128, 8, 128], BF16) for i in range(2)]
        pT = S.ps("pT", [128, 8, 128], BF16)
        ph1 = S.ps("ph1", [128, DE], F32)
        ph3 = S.ps("ph3", [128, DE], F32)
        pa = S.ps("pa", [128, 8, 128], BF16)
        py = [S.ps(f"py{i}", [128, 512], F32) for i in range(2)]
        s1 = [S.sb(f"s1_{i}", [128, DE], F32) for i in range(2)]
        abf = [S.sb(f"ab{i}", [128, DE], BF16) for i in range(2)]
        aT = [S.sb(f"aT{i}", [128, 4, 128], BF16) for i in range(2)]
        yo = [S.sb(f"yo{i}", [128, D], F32) for i in range(2)]
        it = 0
        def gather_w(j):
            bi = j % 2
            for (rows, stg) in ((w1rows, w1s[bi]), (w3rows, w3s[bi]), (w2rows, w2s[bi])):
                S.op("pool", lambda e, rows=rows, stg=stg, j=j: e.indirect_dma_start(out=stg[:, :], out_offset=None, in_=rows,
                                                                                    in_offset=bass.IndirectOffsetOnAxis(ap=WIDXi[:, j:j + 1], axis=0),
                                                                                    bounds_check=S.bc_reg, oob_is_err=False),
                     reads=[WIDXi], writes=[stg], dma=True)

        def cast_w(j):
            bi = j % 2
            S.op("act", lambda e, bi=bi: e.copy(out=w1c[bi][:, :], in_=w1s[bi][:, :]), reads=[w1s[bi]], writes=[w1c[bi]])
            S.op("dve", lambda e, bi=bi: e.tensor_copy(out=w3c[bi][:, :], in_=w3s[bi][:, :]), reads=[w3s[bi]], writes=[w3c[bi]])
            S.op("dve", lambda e, bi=bi: e.tensor_copy(out=w2c[bi][:, :], in_=w2s[bi][:, :]), reads=[w2s[bi]], writes=[w2c[bi]])

        def load_xs(itx):
            x = xs[itx % 2]
            r0 = itx * 128
            S.op("sp", lambda e, x=x, r0=r0: e.dma_start(out=x[:, :], in_=xs_d.t[r0:r0 + 128, :]), reads=[xs_d], writes=[x], dma=True)

        gather_w(0)
        cast_w(0)
        load_xs(0)
        NSB = SLOT // 128
        NSUB = NBLK * NSB

        def stA(it):
            i2 = it % 2
            x = xs[i2]
            for r in range(8):
                S.op("pe", lambda e, r=r, x=x: e.transpose(out=pT[:, r, :], in_=x[:, :].rearrange("p (k r) -> p r k", r=8)[:, r, :], identity=identb[:, :]),
                     reads=[x, identb], writes=[pT])
            S.op("act", lambda e, i2=i2: e.copy(out=xT[i2][:, :, :], in_=pT[:, :, :]), reads=[pT], writes=[xT[i2]])

        def stB(it):
            i2 = it % 2
            bi = (it // NSB) % 2
            for r in range(8):
                S.op("pe", lambda e, r=r, i2=i2, bi=bi: e.matmul(ph1[:, :], lhsT=xT[i2][:, r, :], rhs=w1c[bi][:, r * DE:(r + 1) * DE], start=(r == 0), stop=(r == 7)),
                     reads=[xT[i2], w1c[bi]], writes=[ph1])
            for r in range(8):
                S.op("pe", lambda e, r=r, i2=i2, bi=bi: e.matmul(ph3[:, :], lhsT=xT[i2][:, r, :], rhs=w3c[bi][:, r * DE:(r + 1) * DE], start=(r == 0), stop=(r == 7)),
                     reads=[xT[i2], w3c[bi]], writes=[ph3])
            S.op("act", lambda e, i2=i2: e.activation(out=s1[i2][:, :], in_=ph1[:, :], func=AF.Silu), reads=[ph1], writes=[s1[i2]])
            S.op("dve", lambda e, i2=i2: e.tensor_tensor(out=abf[i2][:, :], in0=ph3[:, :], in1=s1[i2][:, :], op=ALU.mult), reads=[ph3, s1[i2]], writes=[abf[i2]])

        def stC(it):
            i2 = it % 2
            for r in range(4):
                S.op("pe", lambda e, r=r, i2=i2: e.transpose(out=pa[:, r, :], in_=abf[i2][:, :].rearrange("p (k r) -> p r k", r=4)[:, r, :], identity=identb[:, :]),
                     reads=[abf[i2], identb], writes=[pa])
            S.op("act", lambda e, i2=i2: e.copy(out=aT[i2][:, :, :], in_=pa[:, 0:4, :]), reads=[pa], writes=[aT[i2]])

        def stD(it):
            i2 = it % 2
            bi = (it // NSB) % 2
            y = yo[i2]
            r0 = it * 128
            for nch in range(2):
                for r in range(4):
                    S.op("pe", lambda e, r=r, nch=nch, i2=i2, bi=bi: e.matmul(py[nch][:, :], lhsT=aT[i2][:, r, :],
                                                                             rhs=w2c[bi][:, r * D + nch * 512:r * D + (nch + 1) * 512], start=(r == 0), stop=(r == 3)),
                         reads=[aT[i2], w2c[bi]], writes=[py[nch]])
                if nch == 0:
                    S.op("dve", lambda e, y=y, nch=nch: e.tensor_copy(out=y[:, nch * 512:(nch + 1) * 512], in_=py[nch][:, :]), reads=[py[nch]], writes=[y])
                else:
                    S.op("act", lambda e, y=y, nch=nch: e.copy(out=y[:, nch * 512:(nch + 1) * 512], in_=py[nch][:, :]), reads=[py[nch]], writes=[y])
            S.op("sp", lambda e, y=y, r0=r0: e.dma_start(out=yb_d.t[r0:r0 + 128, :], in_=y[:, :]), reads=[y], writes=[yb_d], dma=True)

        load_xs(1)
        stA(0)
        for it in range(NSUB):
            j, sblk = divmod(it, NSB)
            if it + 2 < NSUB:
                pass
            stB(it)
            if it >= 1:
                stD(it - 1)
            if it + 1 < NSUB:
                stA(it + 1)
                if it + 2 < NSUB:
                    load_xs(it + 2)
            if sblk == 0 and j + 1 < NBLK:
                gather_w(j + 1)
            stC(it)
            if sblk == NSB - 1 and j + 1 < NBLK:
                cast_w(j + 1)
        stD(NSUB - 1)
        pC.__exit__(None, None, None)
        pD = S.phase(f"msD{layer}")
        pD.__enter__()
        nrm2 = Norm(S, "n")
        if final_norm:
            fn = bc_load(S, "fn", io["final_norm"].t.rearrange("(o n) -> o n", o=1), D, io["final_norm"])
        xb2 = [S.sb(f"x{i}", [128, D], F32) for i in range(2)]
        ya = [S.sb(f"ya{i}", [128, D], F32) for i in range(3)]
        yb = [S.sb(f"yb{i}", [128, D], F32) for i in range(3)]
        for tt in range(NT):
            x = xb2[tt % 2]
            A = ya[tt % 3]
            B = yb[tt % 3]
            S.op("sp", lambda e, x=x, tt=tt: e.dma_start(out=x[:, :], in_=x_in.t[tt * 128:(tt + 1) * 128, :]), reads=[x_in], writes=[x], dma=True)
            for (ab, dst) in ((0, A), (1, B)):
                S.op("pool", lambda e, ab=ab, dst=dst, tt=tt: e.indirect_dma_start(out=dst[:, :], out_offset=None, in_=yb_d.t[:, :],
                                                                                  in_offset=bass.IndirectOffsetOnAxis(ap=IDXi[:, tt, ab:ab + 1], axis=0)),
                     reads=[IDXi], writes=[dst], dma=True)
            S.op("dve", lambda e, A=A, tt=tt: e.tensor_scalar(out=A[:, :], in0=A[:, :], scalar1=GAB[:, tt, 0:1], scalar2=None, op0=ALU.mult), reads=[A, GAB], writes=[A])
            S.op("dve", lambda e, A=A, B=B, tt=tt: e.scalar_tensor_tensor(out=A[:, :], in0=B[:, :], scalar=GAB[:, tt, 1:2], in1=A[:, :], op0=ALU.mult, op1=ALU.add),
                 reads=[A, B, GAB], writes=[A])
            S.op("dve", lambda e, A=A: e.tensor_tensor(out=A[:, :], in0=A[:, :], in1=m2[:, 2 * D:3 * D], op=ALU.mult), reads=[A, m2], writes=[A])
            S.op("dve", lambda e, A=A, x=x: e.tensor_tensor(out=A[:, :], in0=A[:, :], in1=x[:, :], op=ALU.add), reads=[A, x], writes=[A])
            o = A
            if final_norm:
                o = nrm2.apply(A, fn, None, None, 0, 0, pool_ok=False)
            S.op("act", lambda e, tt=tt, o=o: e.dma_start(out=x_out.t[tt * 128:(tt + 1) * 128, :], in_=o[:, :]), reads=[o], writes=[x_out], dma=True)
        pD.__exit__(None, None, None)


def phase_fourier(S, io, x_in, x_out, mods_d, y_d, f_d):
    with S.phase("fo1"):
        ident = make_ident(S, BF16)
        m1 = load_mod_tiles(S, mods_d, 1, io["norm_mix"], 1, 0)
        cc = S.sb("cc", [128, 2, 2, 256], BF16)
        for tb in range(2):
            S.op("pool", lambda e, tb=tb: e.dma_start(out=cc[:, tb, :, :], in_=io["dftc"].t[tb].rearrange("(k p) n -> p k n", p=128)),
                 reads=[io["dftc"]], writes=[cc], dma=True)
        nrm = Norm(S, "n")
        xb = [S.sb(f"x{i}", [128, D], F32) for i in range(2)]
        hbf = [S.sb(f"h{i}", [128, D], BF16) for i in range(2)]
        hT = [S.sb(f"hT{i}", [128, 8, 128], BF16) for i in range(2)]
        pT = S.ps("pT", [128, 8, 128], BF16)
        pyc = [S.ps(f"pyc{i}", [128, 512], F32) for i in range(4)]
        yb = [S.sb(f"yb{i}", [128, 2, D], BF16) for i in range(2)]
        def front_f1(t):
            x = xb[t % 2]
            S.op("sp", lambda e, x=x, t=t: e.dma_start(out=x[:, :], in_=x_in.t[t * 128:(t + 1) * 128, :]), reads=[x_in], writes=[x], dma=True)
            h = hbf[t % 2]
            nrm.apply(x, m1, m1, h, D, 0)
            transpose_to(S, h, ident, pT, hT[t % 2], 8, "act")
        def back_f1(t):
            y = yb[t % 2]
            for tb in range(2):
                for half in range(2):
                    p = pyc[tb * 2 + half]
                    for gg in range(2):
                        g = half * 2 + gg
                        for k2 in range(2):
                            S.op("pe", lambda e, p=p, gg=gg, g=g, k2=k2, tb=tb, t=t: e.matmul(p[:, gg * 256:(gg + 1) * 256], lhsT=hT[t % 2][:, g * 2 + k2, :],
                                                                                            rhs=cc[:, tb, k2, :], start=(k2 == 0), stop=(k2 == 1)),
                                 reads=[hT[t % 2], cc], writes=[p])
                    if half == 0:
                        S.op("act", lambda e, p=p, y=y, tb=tb, half=half: e.copy(out=y[:, tb, half * 512:(half + 1) * 512], in_=p[:, :]), reads=[p], writes=[y])
                    else:
                        S.op("dve", lambda e, p=p, y=y, tb=tb, half=half: e.tensor_copy(out=y[:, tb, half * 512:(half + 1) * 512], in_=p[:, :]), reads=[p], writes=[y])
            S.op("sp", lambda e, y=y, t=t: e.dma_start(out=y_d.t[:, t * 128:(t + 1) * 128, :].rearrange("c p n -> p c n"), in_=y[:, :, :]),
                 reads=[y], writes=[y_d], dma=True)
        front_f1(0)
        for t in range(NT):
            S.interleave((lambda t=t: front_f1(t + 1)) if t + 1 < NT else None, lambda t=t: back_f1(t))
    for cch in range(2):
        with S.phase(f"fo2{cch}"):
            Y = S.sb("Y", [128, 2, NT, 512], BF16)
            for tb in range(2):
                for q4 in range(4):
                    S.op("sp", lambda e, tb=tb, q4=q4: e.dma_start(
                        out=Y[:, tb, q4 * 8:(q4 + 1) * 8, :],
                        in_=y_d.t[tb, q4 * 1024:(q4 + 1) * 1024, cch * 512:(cch + 1) * 512].rearrange("(t p) n -> p t n", p=128)),
                        reads=[y_d], writes=[Y], dma=True)
            cn = [S.sb(f"cn{i}", [128, 2, NT, 128], BF16) for i in range(2)]
            pP = [S.ps(f"pP{i}", [128, 512], F32) for i in range(2)]
            pQ = [S.ps(f"pQ{i}", [128, 512], F32) for i in range(2)]
            Psb = [S.sb(f"Psb{i}", [128, 512], F32) for i in range(2)]
            fb = [S.sb(f"fb{i}", [128, 512], BF16) for i in range(2)]
            fm = [S.sb(f"fm{i}", [128, 512], BF16) for i in range(2)]
            NH2 = NT // 2
            jf = S.sb("jf", [128, 128], F32)
            S.op("pool", lambda e: e.memset(jf[:, :], 0.0), writes=[jf])
            S.op("pool", lambda e: e.affine_select(out=jf[:, :], in_=jf[:, :], pattern=[[1, 128]], compare_op=ALU.not_equal, fill=1.0, base=-127, channel_multiplier=1),
                 reads=[jf], writes=[jf])
            jb = S.sb("jb", [128, 128], BF16)
            S.op("dve", lambda e: e.tensor_copy(out=jb[:, :], in_=jf[:, :]), reads=[jf], writes=[jb])
            pJ = S.ps("pJ", [128, 512], F32)
            gr = [S.sb(f"gr{i}", [128, 512], BF16) for i in range(2)]

            def load_c(nt):
                c = cn[nt % 2]
                S.op("sp", lambda e, c=c, nt=nt: e.dma_start(out=c[:, :, 0:NT // 2 + 1, :], in_=io["dftn"].t[nt, :, :, 0:NT // 2 + 1, :]), reads=[io["dftn"]], writes=[c], dma=True)

            j1f = S.sb("j1f", [128, 128], F32)
            S.op("pool", lambda e: e.memset(j1f[:, :], 0.0), writes=[j1f])
            S.op("pool", lambda e: e.affine_select(out=j1f[:, :], in_=j1f[:, :], pattern=[[1, 128]], compare_op=ALU.not_equal, fill=1.0, base=-128, channel_multiplier=1),
                 reads=[j1f], writes=[j1f])
            j1 = S.sb("j1", [128, 128], BF16)
            S.op("dve", lambda e: e.tensor_copy(out=j1[:, :], in_=j1f[:, :]), reads=[j1f], writes=[j1])
            j2 = S.sb("j2", [128, 128], BF16)
            S.op("pool", lambda e: e.memset(j2[:, :], 0.0), writes=[j2])
            S.op("pool", lambda e: e.memset(j2[0:1, 0:1], 1.0), reads=[j2], writes=[j2])
            Yp = S.sb("Yp", [128, 2, NH2 + 1, 512], BF16)
            pM = S.ps("pM", [128, 512], F32)
            for t in range(NH2 + 1):
                for tb in range(2):
                    if t == NH2:
                        S.op("pe", lambda e, tb=tb, t=t: e.matmul(pM[:, :], lhsT=j2[:, :], rhs=Y[:, tb, t, :], start=True, stop=True), reads=[j2, Y], writes=[pM])
                        S.op("act", lambda e, tb=tb, t=t: e.copy(out=Yp[:, tb, t, :], in_=pM[:, :]), reads=[pM], writes=[Yp])
                        continue
                    S.op("pe", lambda e, tb=tb, t=t: e.matmul(pM[:, :], lhsT=j1[:, :], rhs=Y[:, tb, NT - 1 - t, :], start=True, stop=(t == 0)), reads=[j1, Y], writes=[pM])
                    if t >= 1:
                        S.op("pe", lambda e, tb=tb, t=t: e.matmul(pM[:, :], lhsT=j2[:, :], rhs=Y[:, tb, NT - t, :], start=False, stop=True), reads=[j2, Y], writes=[pM])
                    S.op("dve", lambda e, tb=tb, t=t: e.tensor_tensor(out=Yp[:, tb, t, :], in0=Y[:, tb, t, :], in1=pM[:, :],
                                                                      op=(ALU.add if tb == 0 else ALU.subtract)), reads=[Y, pM], writes=[Yp])
            NTC = NH2 + 1
            load_c(0)
            for nt in range(NH2 + 1):
                c = cn[nt % 2]
                if nt + 1 <= NH2:
                    load_c(nt + 1)
                p, q = pP[nt % 2], pQ[nt % 2]
                for t in range(NTC):
                    S.op("pe", lambda e, c=c, p=p, t=t: e.matmul(p[:, :], lhsT=c[:, 0, t, :], rhs=Yp[:, 0, t, :], start=(t == 0), stop=(t == NTC - 1)),
                         reads=[c, Yp], writes=[p])
                for t in range(NTC):
                    S.op("pe", lambda e, c=c, q=q, t=t: e.matmul(q[:, :], lhsT=c[:, 1, t, :], rhs=Yp[:, 1, t, :], start=(t == 0), stop=(t == NTC - 1)),
                         reads=[c, Yp], writes=[q])
                ps_ = Psb[nt % 2]
                f = fb[nt % 2]
                g = fm[nt % 2]
                S.op("act", lambda e, ps_=ps_, p=p: e.copy(out=ps_[:, :], in_=p[:, :]), reads=[p], writes=[ps_])
                S.op("dve", lambda e, ps_=ps_, q=q, f=f: e.tensor_tensor(out=f[:, :], in0=ps_[:, :], in1=q[:, :], op=ALU.add), reads=[ps_, q], writes=[f])
                if nt < NH2:
                    S.op("sp", lambda e, f=f, nt=nt: e.dma_start(out=f_d.t[nt * 128:(nt + 1) * 128, cch * 512:(cch + 1) * 512], in_=f[:, :]),
                         reads=[f], writes=[f_d], dma=True)
                    S.op("dve", lambda e, ps_=ps_, q=q, g=g: e.tensor_tensor(out=g[:, :], in0=ps_[:, :], in1=q[:, :], op=ALU.subtract), reads=[ps_, q], writes=[g])
                    g2 = gr[nt % 2]
                    S.op("pe", lambda e, g=g: e.matmul(pJ[:, :], lhsT=jb[:, :], rhs=g[:, :], start=True, stop=True), reads=[jb, g], writes=[pJ])
                    S.op("act", lambda e, g2=g2: e.copy(out=g2[:, :], in_=pJ[:, :]), reads=[pJ], writes=[g2])
                    npart = 127 if nt == 0 else 128
                    r0 = T - nt * 128 - 127
                    S.op("sp", lambda e, g2=g2, r0=r0, npart=npart: e.dma_start(out=f_d.t[r0:r0 + npart, cch * 512:(cch + 1) * 512], in_=g2[0:npart, :]),
                         reads=[g2], writes=[f_d], dma=True)
                else:
                    S.op("sp", lambda e, f=f, nt=nt: e.dma_start(out=f_d.t[nt * 128:nt * 128 + 1, cch * 512:(cch + 1) * 512], in_=f[0:1, :]),
                         reads=[f], writes=[f_d], dma=True)
    with S.phase("fo3"):
        ident = make_ident(S, BF16)
        m1 = load_mod_tiles(S, mods_d, 1, io["norm_mix"], 1, 0)
        wfo = S.sb("wfo", [128, 8, D], BF16)
        for hh in range(2):
            S.op("pool", lambda e, hh=hh: e.dma_start(out=wfo[:, :, hh * 512:(hh + 1) * 512],
                                                      in_=io["fourier_w_o"].t[:, hh * 512:(hh + 1) * 512].rearrange("(k p) n -> p k n", p=128)),
                 reads=[io["fourier_w_o"]], writes=[wfo], dma=True)
        fb = [S.sb(f"f{i}", [128, D], BF16) for i in range(2)]
        fT = [S.sb(f"fT{i}", [128, 8, 128], BF16) for i in range(2)]
        xb = [S.sb(f"x{i}", [128, D], F32) for i in range(2)]
        xo = [S.sb(f"xo{i}", [128, D], F32) for i in range(2)]
        pT = S.ps("pT", [128, 8, 128], BF16)
        py = [S.ps(f"py{i}", [128, 512], F32) for i in range(2)]
        tmpy = S.sb("tmpy", [128, 512], F32)
        def front_f3(t):
            f = fb[t % 2]
            x = xb[t % 2]
            S.op("sp", lambda e, f=f, t=t: e.dma_start(out=f[:, :], in_=f_d.t[t * 128:(t + 1) * 128, :]), reads=[f_d], writes=[f], dma=True)
            S.op("sp", lambda e, x=x, t=t: e.dma_start(out=x[:, :], in_=x_in.t[t * 128:(t + 1) * 128, :]), reads=[x_in], writes=[x], dma=True)
            transpose_to(S, f, ident, pT, fT[t % 2], 8, "act")
        def back_f3(t):
            x = xb[t % 2]
            o = xo[t % 2]
            for nch in range(2):
                for k in range(8):
                    S.op("pe", lambda e, k=k, nch=nch, t=t: e.matmul(py[nch][:, :], lhsT=fT[t % 2][:, k, :], rhs=wfo[:, k, nch * 512:(nch + 1) * 512],
                                                                     start=(k == 0), stop=(k == 7)), reads=[fT[t % 2], wfo], writes=[py[nch]])
                S.op("dve", lambda e, nch=nch: e.tensor_tensor(out=tmpy[:, :], in0=py[nch][:, :], in1=m1[:, 2 * D + nch * 512:2 * D + (nch + 1) * 512], op=ALU.mult),
                     reads=[py[nch], m1], writes=[tmpy])
                S.op("pool", lambda e, nch=nch, o=o, x=x: e.tensor_tensor(out=o[:, nch * 512:(nch + 1) * 512], in0=tmpy[:, :], in1=x[:, nch * 512:(nch + 1) * 512], op=ALU.add),
                     reads=[tmpy, x], writes=[o])
            S.op("sp", lambda e, t=t, o=o: e.dma_start(out=x_out.t[t * 128:(t + 1) * 128, :], in_=o[:, :]), reads=[o], writes=[x_out], dma=True)
        front_f3(0)
        for t in range(NT):
            S.interleave((lambda t=t: front_f3(t + 1)) if t + 1 < NT else None, lambda t=t: back_f3(t))


IN_SPECS = {
    "x": ([T, D], F32), "c": ([D], F32), "ctx": ([CTX, D], F32), "c_ctx": ([D], F32),
    "w_mod": ([2, D, 6 * D], F32), "b_mod": ([2, 6 * D], F32), "norm_mix": ([2, D], F32), "norm_ffn": ([2, D], F32),
    "attn_w_qkv": ([D, 1536], F32), "attn_q_norm": ([1, HD], F32), "attn_k_norm": ([1, HD], F32), "attn_w_o": ([D, D], F32),
    "fourier_w_o": ([D, D], F32), "moe_w_rg": ([2, D, 4], F32), "moe_b_rg": ([2, 4], F32), "moe_w_re": ([2, D, NE], F32),
    "moe_b_re": ([2, NE], F32), "moe_w1": ([2, NE, D, DE], F32), "moe_w3": ([2, NE, D, DE], F32), "moe_w2": ([2, NE, DE, D], F32),
    "final_norm": ([D], F32),
    "tri": ([128, 128], F32), "thr": ([1, 16 + 64], F32), "pidx": ([128, 1], F32),
    "rope": ([2, T, HD], F32), "dftc": ([2, 256, 256], F32), "dftn": ([NT, 128, 2, NT, 128], BF16),
}


def build_program(phases=("mod", "att", "moe0", "fou", "moe1"), only=None):
    nc = bass.Bass("TRN2", target_bir_lowering=False)
    with contextlib.ExitStack() as st:
        S = Sched(nc, st)
        io = {k: S.dram(k, shp, dt, kind="ExternalInput") for k, (shp, dt) in IN_SPECS.items() if only is None or k in only}
        out = S.dram("out", [T, D], F32, kind="ExternalOutput")
        mods_d = S.dram("mods_scr", [2, 6 * D], F32)
        modc_d = S.dram("modc_scr", [1, 2 * D], F32)
        x1 = S.dram("x1_scr", [T, D], F32)
        x2 = S.dram("x2_scr", [T, D], F32)
        x3 = S.dram("x3_scr", [T, D], F32)
        y_d = S.dram("y_scr", [2, T, D], BF16)
        f_d = S.dram("f_scr", [T, D], BF16)
        scr = (S.dram("h2_scr", [T, D], BF16), S.dram("xs_scr", [NBLK * SLOT, D], BF16), S.dram("yb_scr", [NBLK * SLOT, D], F32))
        DENSE = os.environ.get("MOE_DENSE", "0") == "1"
        last = phases[-1]
        if "mod" in phases:
            phase_mods(S, io, 0, mods_d, modc_d)
            phase_mods(S, io, 1, mods_d, None)
        if "att" in phases:
            phase_attn(S, io, io["x"], out if last == "att" else x1, mods_d, modc_d)
        if "moe0" in phases:
            if DENSE:
                phase_moe(S, io, 0, x1 if "att" in phases else io["x"], out if last == "moe0" else x2, mods_d)
            else:
                phase_moe_sorted(S, io, 0, x1 if "att" in phases else io["x"], out if last == "moe0" else x2, mods_d, scr)
        if "fou" in phases:
            phase_fourier(S, io, x2 if "moe0" in phases else io["x"], out if last == "fou" else x3, mods_d, y_d, f_d)
        if "moe1" in phases:
            if DENSE:
                phase_moe(S, io, 1, x3 if "fou" in phases else io["x"], out, mods_d, final_norm=True)
            else:
                phase_moe_sorted(S, io, 1, x3 if "fou" in phases else io["x"], out, mods_d, scr, final_norm=True)
        if last == "mod":
            with S.phase("dbg"):
                t = S.sb("t", [2, 6 * D], F32)
                dma(S, "sp", t, t[:, :], mods_d, mods_d.t[:, :])
                dma(S, "sp", out, out.t[0:12, :].rearrange("(a r) n -> a (r n)", a=2), t, t[:, :])
        S.finish()
    return nc


def host_constants():
    half = 16
    inv = (10000.0 ** (-np.arange(half, dtype=np.float32) / half)).astype(np.float32)
    n = np.arange(T)
    ang_r = (n // 64).astype(np.float32)[:, None] * inv
    ang_c = (n % 64).astype(np.float32)[:, None] * inv
    cr, sr, ccs, scs = np.cos(ang_r), np.sin(ang_r), np.cos(ang_c), np.sin(ang_c)
    cos64 = np.concatenate([cr, cr, ccs, ccs], axis=1)
    sin64 = np.concatenate([-sr, sr, -scs, scs], axis=1)
    rope = np.stack([cos64, sin64]).astype(np.float32)
    k = np.arange(256)
    a = 2 * np.pi * ((k[:, None] * k[None, :]) % 256) / 256.0
    dftc = np.stack([np.cos(a), np.sin(a)]).astype(np.float32) / 32.0
    m = np.arange(T, dtype=np.int64)
    an = 2 * np.pi * ((m[:, None] * m[None, :]) % T) / float(T)
    cn = (np.cos(an) / 32.0).astype(np.float32)
    sn = (-np.sin(an) / 32.0).astype(np.float32)
    tb = np.stack([cn, sn])
    tb = tb.reshape(2, NT, 128, NT, 128).transpose(3, 2, 0, 1, 4)
    dftn = np.ascontiguousarray(tb).astype(ml_dtypes.bfloat16)
    tri = (np.arange(128)[:, None] < np.arange(128)[None, :]).astype(np.float32)
    thr = np.concatenate([256.0 * np.arange(16), float(SLOT) * np.arange(NBLK)]).astype(np.float32)[None, :]
    pidx = np.arange(128, dtype=np.float32)[:, None]
    return {"rope": rope, "dftc": dftc, "dftn": dftn, "tri": tri, "thr": thr, "pidx": pidx}


def make_in_maps(inputs, consts):
    B = inputs["x"].shape[0]
    f = lambda a: np.ascontiguousarray(np.asarray(a, dtype=np.float32))
    shared = {
        "c_ctx": f(inputs["c_ctx"]), "w_mod": f(inputs["w_mod"]), "b_mod": f(inputs["b_mod"]),
        "norm_mix": f(inputs["norm_mix"]), "norm_ffn": f(inputs["norm_ffn"]), "attn_w_qkv": f(inputs["attn_w_qkv"][0]),
        "attn_q_norm": f(inputs["attn_q_norm"]), "attn_k_norm": f(inputs["attn_k_norm"]), "attn_w_o": f(inputs["attn_w_o"][0]),
        "fourier_w_o": f(inputs["fourier_w_o"][0]), "moe_w_rg": f(inputs["moe_w_rg"]), "moe_b_rg": f(inputs["moe_b_rg"]),
        "moe_w_re": f(inputs["moe_w_re"]), "moe_b_re": f(inputs["moe_b_re"]), "moe_w1": f(inputs["moe_w1"]),
        "moe_w3": f(inputs["moe_w3"]), "moe_w2": f(inputs["moe_w2"]), "final_norm": f(inputs["final_norm"]),
    }
    shared.update(consts)
    maps = []
    for b in range(B):
        m = dict(shared)
        m["x"] = f(inputs["x"][b])
        m["c"] = f(inputs["c"][b])
        m["ctx"] = f(inputs["ctx"][b])
        maps.append(m)
    return maps


def kernel(**inputs):
    consts = host_constants()
    maps = make_in_maps(inputs, consts)
    nc = build_program()
    res = run_bass_kernel_spmd(nc, maps, core_ids=list(range(len(maps))))
    return np.stack([np.asarray(r["out"], dtype=np.float32) for r in res.results], axis=0)
```
